# Optimizing a Trainium2 kernel written in Bass

```python
import math
import jax, jax.numpy as jnp
from jax import lax
import numpy as np

D_MODEL = 1024
BATCH = 4
SEQ = 8192
DEPTH = 2

ROPE_THETA = 500000.0
Q_BLOCK = 128
LN_EPS = 1e-5
POOL_WIDTH = 512
POOL_WINDOWS = (2, 4, 8, 16)
POOL_GROUP = POOL_WIDTH // 4
CONV_WIDTH = 512
CONV_K = 31
DSA_HEADS = 8
DSA_HEAD_DIM = 64
DSA_ROT = DSA_HEAD_DIM // 4
IDX_HEADS = 8
IDX_DIM = 32
IDX_ROT = IDX_DIM // 4
DSA_TOPK = 256
MLA_HEADS = 8
MLA_NOPE = 64
MLA_ROPE = 32
MLA_V = 64
MLA_Q_RANK = 384
MLA_KV_RANK = 256
N_BRANCH = 4
N_EXPERTS = 32
TOP_K = 4
D_FF = 1024
SWIGLU_ALPHA = 1.702
SWIGLU_LIMIT = 7.0
MOE_BLOCK = 512
DEEPNORM_ALPHA = (2 * DEPTH) ** 0.25
DEEPNORM_BETA = (8 * DEPTH) ** -0.25
SPLITS = (POOL_WIDTH, 2 * CONV_WIDTH,
          DSA_HEADS * DSA_HEAD_DIM, DSA_HEADS * DSA_HEAD_DIM, DSA_HEADS * DSA_HEAD_DIM,
          IDX_HEADS * IDX_DIM, IDX_DIM, IDX_HEADS,
          MLA_Q_RANK, MLA_KV_RANK, MLA_ROPE,
          N_BRANCH * D_MODEL)
D_IN = sum(SPLITS)

kernel_name = 'hybrid_pool_conv_dsa_mla_moe'

F32 = jnp.float32


def _layernorm(x, g, b):
    xf = x.astype(F32)
    mu = jnp.mean(xf, axis=-1, keepdims=True)
    var = jnp.mean(jnp.square(xf - mu), axis=-1, keepdims=True)
    y = (xf - mu) * lax.rsqrt(var + LN_EPS) * g.astype(F32) + b.astype(F32)
    return y.astype(x.dtype)


def _rmsnorm(x, g):
    xf = x.astype(F32)
    y = xf * lax.rsqrt(jnp.mean(jnp.square(xf), axis=-1, keepdims=True) + LN_EPS) * g.astype(F32)
    return y.astype(x.dtype)


def _rope_tables(positions, rot):
    inv = jnp.power(ROPE_THETA, -jnp.arange(0, rot, 2, dtype=F32) / rot)
    ang = positions.astype(F32)[..., None] * inv
    return jnp.cos(ang)[:, :, None, :], jnp.sin(ang)[:, :, None, :]


def _apply_rope(x, cos, sin, rot):
    xr = x[..., :rot].astype(F32)
    x1, x2 = xr[..., :rot // 2], xr[..., rot // 2:]
    r = jnp.concatenate([x1 * cos - x2 * sin, x2 * cos + x1 * sin], axis=-1)
    return jnp.concatenate([r.astype(x.dtype), x[..., rot:]], axis=-1)


def _pool_mixer(u, pool_w, pool_scale, pool_out):
    B, S, _ = u.shape
    ug = u.astype(F32).reshape(B, S, len(POOL_WINDOWS), POOL_GROUP)
    cs = jnp.cumsum(ug, axis=1)
    pos = jnp.arange(S)
    outs = []
    for gi, w in enumerate(POOL_WINDOWS):
        c = cs[:, :, gi]
        lag = jnp.pad(c[:, :S - w], ((0, 0), (w, 0), (0, 0)))
        mean = (c - lag) / jnp.minimum(pos + 1, w).astype(F32)[None, :, None]
        outs.append(mean - ug[:, :, gi])
    pooled = jnp.stack(outs, axis=2).astype(u.dtype)
    mixed = jnp.einsum('bsgc,gcd->bsgd', pooled, pool_w).reshape(B, S, POOL_WIDTH) * pool_scale
    return mixed @ pool_out


def _conv_mixer(u, conv_w, conv_b, ln_g, ln_b, conv_out):
    a, gate = jnp.split(u, 2, axis=-1)
    h = a * jax.nn.sigmoid(gate)
    h = lax.conv_general_dilated(h, conv_w[:, None, :], window_strides=(1,),
                                 padding=[(CONV_K - 1, 0)],
                                 dimension_numbers=('NWC', 'WIO', 'NWC'),
                                 feature_group_count=CONV_WIDTH) + conv_b
    h = jax.nn.silu(_layernorm(h, ln_g, ln_b))
    return h @ conv_out


def _dsa_mixer(q, k, v, qi, ki, wi, rope_c, rope_i, dsa_out):
    B, S, _ = q.shape
    q = _apply_rope(q.reshape(B, S, DSA_HEADS, DSA_HEAD_DIM), *rope_c, DSA_ROT)
    k = _apply_rope(k.reshape(B, S, DSA_HEADS, DSA_HEAD_DIM), *rope_c, DSA_ROT)
    v = v.reshape(B, S, DSA_HEADS, DSA_HEAD_DIM)
    qi = _apply_rope(qi.reshape(B, S, IDX_HEADS, IDX_DIM), *rope_i, IDX_ROT)
    ki = _apply_rope(ki[:, :, None, :], *rope_i, IDX_ROT)[:, :, 0].astype(F32)
    wi = wi.astype(F32) * IDX_HEADS ** -0.5
    n_sel = min(DSA_TOPK, S // 4)
    nb = S // Q_BLOCK
    key_pos = jnp.arange(S)

    def to_blocks(t):
        return jnp.moveaxis(t.reshape(B, nb, Q_BLOCK, *t.shape[2:]), 1, 0)

    def block(args):
        qb, qib, wb, i = args
        qpos = i * Q_BLOCK + jnp.arange(Q_BLOCK)
        logits = jnp.einsum('bqhd,bsd->bqhs', qib.astype(F32), ki) * IDX_DIM ** -0.5
        score = jnp.einsum('bqh,bqhs->bqs', wb, jax.nn.relu(logits))
        causal = key_pos[None, :] <= qpos[:, None]
        score = jnp.where(causal[None], score, -jnp.inf)
        _, sel = lax.top_k(score, n_sel)
        valid = sel <= qpos[None, :, None]
        k_sel = jax.vmap(lambda kb, ib: kb[ib])(k, sel)
        v_sel = jax.vmap(lambda vb, ib: vb[ib])(v, sel)
        s = jnp.einsum('bqhd,bqnhd->bqhn', qb, k_sel).astype(F32) * DSA_HEAD_DIM ** -0.5
        s = jnp.where(valid[:, :, None, :], s, -jnp.inf)
        p = jax.nn.softmax(s, axis=-1).astype(v.dtype)
        return jnp.einsum('bqhn,bqnhd->bqhd', p, v_sel)

    o = lax.map(block, (to_blocks(q), to_blocks(qi), to_blocks(wi), jnp.arange(nb)))
    o = jnp.moveaxis(o, 0, 1).reshape(B, S, DSA_HEADS * DSA_HEAD_DIM)
    return o @ dsa_out


def _mla_mixer(cq, ckv, kr, q_norm, kv_norm, wuq, wuk, wuv, rope_m, mla_out):
    B, S, _ = cq.shape
    q = (_rmsnorm(cq, q_norm) @ wuq).reshape(B, S, MLA_HEADS, MLA_NOPE + MLA_ROPE)
    q_nope = q[..., :MLA_NOPE]
    q_rope = _apply_rope(q[..., MLA_NOPE:], *rope_m, MLA_ROPE)
    c = _rmsnorm(ckv, kv_norm)
    k_nope = (c @ wuk).reshape(B, S, MLA_HEADS, MLA_NOPE)
    v = (c @ wuv).reshape(B, S, MLA_HEADS, MLA_V)
    k_rope = _apply_rope(kr[:, :, None, :], *rope_m, MLA_ROPE)[:, :, 0]
    scale = (MLA_NOPE + MLA_ROPE) ** -0.5
    outs = []
    for i in range(S // Q_BLOCK):
        lo, hi = i * Q_BLOCK, (i + 1) * Q_BLOCK
        s = (jnp.einsum('bqhd,bkhd->bhqk', q_nope[:, lo:hi], k_nope[:, :hi])
             + jnp.einsum('bqhr,bkr->bhqk', q_rope[:, lo:hi], k_rope[:, :hi]))
        mask = jnp.arange(hi)[None, :] <= (lo + jnp.arange(Q_BLOCK))[:, None]
        s = jnp.where(mask, s.astype(F32) * scale, -jnp.inf)
        p = jax.nn.softmax(s, axis=-1).astype(v.dtype)
        outs.append(jnp.einsum('bhqk,bkhd->bqhd', p, v[:, :hi]))
    o = jnp.concatenate(outs, axis=1).reshape(B, S, MLA_HEADS * MLA_V)
    return o @ mla_out


def _mixing_block(x, ropes, w_in, pool_w, pool_scale, pool_out, conv_w, conv_b, conv_ln_g, conv_ln_b,
                  conv_out, dsa_out, mla_q_norm, mla_kv_norm, mla_wuq, mla_wuk, mla_wuv, mla_out, w_o):
    B, S, D = x.shape
    proj = x @ w_in
    offsets = [int(o) for o in np.cumsum(SPLITS)[:-1]]
    (u_pool, u_conv, c_q, c_k, c_v, i_q, i_k, i_w, m_q, m_kv, m_kr, g_all) = jnp.split(proj, offsets, axis=-1)
    rope_c, rope_i, rope_m = ropes
    y_a = _pool_mixer(u_pool, pool_w, pool_scale, pool_out)
    y_b = _conv_mixer(u_conv, conv_w, conv_b, conv_ln_g, conv_ln_b, conv_out)
    y_c = _dsa_mixer(c_q, c_k, c_v, i_q, i_k, i_w, rope_c, rope_i, dsa_out)
    y_d = _mla_mixer(m_q, m_kv, m_kr, mla_q_norm, mla_kv_norm, mla_wuq, mla_wuk, mla_wuv, rope_m, mla_out)
    gates = jax.nn.sigmoid(g_all.reshape(B, S, N_BRANCH, D))
    merged = gates[:, :, 0] * y_a + gates[:, :, 1] * y_b + gates[:, :, 2] * y_c + gates[:, :, 3] * y_d
    return merged @ w_o


def _moe(h, router_w, router_b, w_gu, b_gu, w_d, b_d):
    B, S, D = h.shape
    n_tok = B * S
    xt = h.reshape(n_tok, D)
    logits = (xt @ router_w + router_b).astype(F32)
    top_val, top_idx = lax.top_k(logits, TOP_K)
    gate = jax.nn.softmax(top_val, axis=-1)
    n_assign = n_tok * TOP_K
    flat_e = top_idx.reshape(n_assign)
    order = jnp.argsort(flat_e)
    e_sorted = flat_e[order]
    tok_sorted = order // TOP_K
    gate_sorted = gate.reshape(n_assign)[order].astype(h.dtype)
    counts = jnp.bincount(flat_e, length=N_EXPERTS)
    padded = (counts + MOE_BLOCK - 1) // MOE_BLOCK * MOE_BLOCK
    pad_end = jnp.cumsum(padded)
    pad_start = pad_end - padded
    grp_start = jnp.cumsum(counts) - counts
    dest = pad_start[e_sorted] + (jnp.arange(n_assign) - grp_start[e_sorted])
    n_blocks = -(-n_assign // MOE_BLOCK) + N_EXPERTS
    buf = jnp.zeros((n_blocks * MOE_BLOCK, D), h.dtype).at[dest].set(xt[tok_sorted])
    blk_expert = jnp.minimum(jnp.searchsorted(pad_end, jnp.arange(n_blocks) * MOE_BLOCK, side='right'),
                             N_EXPERTS - 1)

    def expert_block(args):
        xb, e = args
        gu = xb @ w_gu[e] + b_gu[e]
        g = jnp.minimum(gu[:, :D_FF], SWIGLU_LIMIT)
        lin = jnp.clip(gu[:, D_FF:], -SWIGLU_LIMIT, SWIGLU_LIMIT)
        act = g * jax.nn.sigmoid(SWIGLU_ALPHA * g) * (lin + 1)
        return act @ w_d[e] + b_d[e]

    y_buf = lax.map(expert_block, (buf.reshape(n_blocks, MOE_BLOCK, D), blk_expert)).reshape(-1, D)
    y = y_buf[dest] * gate_sorted[:, None]
    out = jnp.zeros((n_tok, D), h.dtype).at[tok_sorted].add(y)
    return out.reshape(B, S, D)


def setup_inputs(seed: int = 0) -> dict:
    key = jax.random.key(seed)
    ks = iter(jax.random.split(key, 40))
    L, D, E = DEPTH, D_MODEL, N_EXPERTS

    def nrm(shape, scale):
        return jax.random.normal(next(ks), shape, F32) * scale

    def gain(shape):
        return 1.0 + nrm(shape, 0.02)

    x = nrm((BATCH, SEQ, D), 1.0)
    offset = jax.random.randint(next(ks), (BATCH, 1), 0, 4096, dtype=jnp.int32)
    positions = offset + jnp.arange(SEQ, dtype=jnp.int32)[None, :]
    return {
        'x': x,
        'positions': positions,
        'w_in': nrm((L, D, D_IN), D ** -0.5),
        'pool_w': nrm((L, 4, POOL_GROUP, POOL_GROUP), POOL_GROUP ** -0.5),
        'pool_scale': gain((L, POOL_WIDTH)),
        'pool_out': nrm((L, POOL_WIDTH, D), POOL_WIDTH ** -0.5),
        'conv_w': nrm((L, CONV_K, CONV_WIDTH), CONV_K ** -0.5),
        'conv_b': nrm((L, CONV_WIDTH), 0.02),
        'conv_ln_g': gain((L, CONV_WIDTH)),
        'conv_ln_b': nrm((L, CONV_WIDTH), 0.02),
        'conv_out': nrm((L, CONV_WIDTH, D), CONV_WIDTH ** -0.5),
        'dsa_out': nrm((L, DSA_HEADS * DSA_HEAD_DIM, D), (DSA_HEADS * DSA_HEAD_DIM) ** -0.5),
        'mla_q_norm': gain((L, MLA_Q_RANK)),
        'mla_kv_norm': gain((L, MLA_KV_RANK)),
        'mla_wuq': nrm((L, MLA_Q_RANK, MLA_HEADS * (MLA_NOPE + MLA_ROPE)), MLA_Q_RANK ** -0.5),
        'mla_wuk': nrm((L, MLA_KV_RANK, MLA_HEADS * MLA_NOPE), MLA_KV_RANK ** -0.5),
        'mla_wuv': nrm((L, MLA_KV_RANK, MLA_HEADS * MLA_V), MLA_KV_RANK ** -0.5),
        'mla_out': nrm((L, MLA_HEADS * MLA_V, D), (MLA_HEADS * MLA_V) ** -0.5),
        'w_o': nrm((L, D, D), D ** -0.5 * DEEPNORM_BETA),
        'ln1_g': gain((L, D)),
        'ln1_b': nrm((L, D), 0.02),
        'router_w': nrm((L, D, E), D ** -0.5),
        'router_b': nrm((L, E), 0.01),
        'exp_w_gu': nrm((L, E, D, 2 * D_FF), D ** -0.5),
        'exp_b_gu': nrm((L, E, 2 * D_FF), 0.02),
        'exp_w_d': nrm((L, E, D_FF, D), D_FF ** -0.5 * DEEPNORM_BETA),
        'exp_b_d': nrm((L, E, D), 0.02),
        'ln2_g': gain((L, D)),
        'ln2_b': nrm((L, D), 0.02),
    }


def reference(x, positions, w_in, pool_w, pool_scale, pool_out, conv_w, conv_b, conv_ln_g, conv_ln_b,
              conv_out, dsa_out, mla_q_norm, mla_kv_norm, mla_wuq, mla_wuk, mla_wuv, mla_out, w_o,
              ln1_g, ln1_b, router_w, router_b, exp_w_gu, exp_b_gu, exp_w_d, exp_b_d, ln2_g, ln2_b):
    ropes = (_rope_tables(positions, DSA_ROT), _rope_tables(positions, IDX_ROT),
             _rope_tables(positions, MLA_ROPE))
    for l in range(DEPTH):
        y = _mixing_block(x, ropes, w_in[l], pool_w[l], pool_scale[l], pool_out[l], conv_w[l], conv_b[l],
                          conv_ln_g[l], conv_ln_b[l], conv_out[l], dsa_out[l], mla_q_norm[l],
                          mla_kv_norm[l], mla_wuq[l], mla_wuk[l], mla_wuv[l], mla_out[l], w_o[l])
        x = _layernorm(DEEPNORM_ALPHA * x + y, ln1_g[l], ln1_b[l])
        y = _moe(x, router_w[l], router_b[l], exp_w_gu[l], exp_b_gu[l], exp_w_d[l], exp_b_d[l])
        x = _layernorm(DEEPNORM_ALPHA * x + y, ln2_g[l], ln2_b[l])
    return x
```

```python
import math
import numpy as np
import ml_dtypes
from contextlib import ExitStack
import concourse.bass as bass
import concourse.mybir as mybir
from concourse.bass_utils import run_bass_kernel_spmd

F32 = mybir.dt.float32
BF16 = mybir.dt.bfloat16
I32 = mybir.dt.int32
AF = mybir.ActivationFunctionType
ALU = mybir.AluOpType
AX = mybir.AxisListType
NPBF = ml_dtypes.bfloat16

ENGS = ("pe", "act", "dve", "pool", "sp")
N_DMA_SEMS = 6


class Prog:
    def __init__(self, name="k"):
        self.nc = bass.Bass("TRN2", target_bir_lowering=False)
        self.es = ExitStack()
        self.ops = []
        self.n_sb = 0
        self.out_keys = []

    def dram_in(self, name, shape, dt):
        return self.nc.dram_tensor(name, list(shape), dt, kind="ExternalInput").ap()

    def dram_out(self, name, shape, dt):
        return self.nc.dram_tensor(name, list(shape), dt, kind="ExternalOutput").ap()

    def dram_tmp(self, name, shape, dt):
        return self.nc.dram_tensor(name, list(shape), dt, kind="Internal").ap()

    def sb(self, shape, dt, name=None):
        self.n_sb += 1
        return self.es.enter_context(self.nc.sbuf_tensor(name or f"sb{self.n_sb}", list(shape), dt))

    def ps(self, shape, dt=F32, name=None):
        self.n_sb += 1
        return self.es.enter_context(self.nc.psum_tensor(name or f"ps{self.n_sb}", list(shape), dt))

    def _init_emit(self):
        nc = self.nc
        self.engobj = {"pe": nc.tensor, "act": nc.scalar, "dve": nc.vector, "pool": nc.gpsimd, "sp": nc.sync}
        self.sems = {e: self.es.enter_context(nc.semaphore(f"s_{e}")) for e in ENGS}
        self.dsems = {}
        self.cnt = {e: 0 for e in ENGS}
        self.dcnt = {}
        self.dma_n = {e: 0 for e in ENGS}
        self.slot_last = {}
        self.lastw = {}
        self.readers = {}
        self.known = {e: {} for e in ENGS}
        self.n_wait = 0
        self.n_ops = 0

    def op(self, eng, fn, reads=(), writes=(), dma=False):
        if not hasattr(self, "engobj"):
            self._init_emit()
        if getattr(self, "dead", False):
            return
        E = self.engobj[eng]
        need = []
        for k in reads:
            sg = self.lastw.get(k)
            if sg is not None:
                need.append(sg)
        for k in writes:
            sg = self.lastw.get(k)
            if sg is not None:
                need.append(sg)
            need.extend(self.readers.get(k, ()))
        slot = None
        if dma:
            slot = (eng, self.dma_n[eng] % N_DMA_SEMS)
            self.dma_n[eng] += 1
            if slot not in self.dsems:
                self.dsems[slot] = self.es.enter_context(self.nc.semaphore(f"d_{eng}{slot[1]}"))
            if slot in self.slot_last:
                need.append(self.slot_last[slot])
        best = {}
        for sg in need:
            name, h, v, src, sdma = sg
            if (not dma) and (not sdma) and src == "pe" and eng == "pe":
                continue
            if self.known[eng].get(name, 0) >= v:
                continue
            if name not in best or best[name][2] < v:
                best[name] = sg
        for name, sg in best.items():
            E.wait_ge(sg[1], sg[2])
            self.known[eng][name] = sg[2]
            self.n_wait += 1
        inst = fn(E)
        self.n_ops += 1
        if dma:
            self.dcnt[slot] = self.dcnt.get(slot, 0) + 16
            inst.then_inc(self.dsems[slot], 16)
            mysig = (f"d_{slot}", self.dsems[slot], self.dcnt[slot], eng, True)
            self.slot_last[slot] = mysig
        else:
            self.cnt[eng] += 1
            inst.then_inc(self.sems[eng], 1)
            mysig = (f"s_{eng}", self.sems[eng], self.cnt[eng], eng, False)
        for k in writes:
            self.lastw[k] = mysig
            self.readers[k] = []
        for k in reads:
            if k not in writes:
                self.readers.setdefault(k, []).append(mysig)

    def pe(self, fn, r=(), w=()):
        self.op("pe", fn, r, w)

    def act(self, fn, r=(), w=()):
        self.op("act", fn, r, w)

    def dve(self, fn, r=(), w=()):
        self.op("dve", fn, r, w)

    def pool(self, fn, r=(), w=()):
        self.op("pool", fn, r, w)

    def dma(self, q, fn, r=(), w=()):
        self.op(q, fn, r, w, dma=True)

    def barrier(self):
        if not hasattr(self, "engobj"):
            self._init_emit()
        for e in ENGS:
            E = self.engobj[e]
            for f in ("pe", "act", "dve", "pool"):
                if self.cnt[f] and self.known[e].get(f"s_{f}", 0) < self.cnt[f]:
                    E.wait_ge(self.sems[f], self.cnt[f])
                    self.known[e][f"s_{f}"] = self.cnt[f]
            for slot, v in self.dcnt.items():
                nm = f"d_{slot}"
                if self.known[e].get(nm, 0) < v:
                    E.wait_ge(self.dsems[slot], v)
                    self.known[e][nm] = v

    def finish(self):
        nc = self.nc
        for slot, v in self.dcnt.items():
            nc.sync.wait_ge(self.dsems[slot], v)
        for e in ("pe", "act", "dve", "pool"):
            if self.cnt[e]:
                nc.sync.wait_ge(self.sems[e], self.cnt[e])
        self.stats = dict(n_ops=self.n_ops, n_wait=self.n_wait, cnt=dict(self.cnt))
        self.es.close()
        return nc


T, H, TT, NT = 4096, 32, 512, 8
EPS = 1e-5
THETA = 500000.0
O_POOL, O_CA, O_CG, O_CQ, O_CK, O_CV, O_IQ, O_IK, O_IW, O_MQ, O_MKV, O_MKR, O_G = (
    0, 512, 1024, 1536, 2048, 2560, 3072, 3328, 3360, 3368, 3752, 4008, 4040)
PI = math.pi


class Ring:
    def __init__(self, P, n, shape, dt, name):
        self.t = [P.sb(shape, dt, name=f"{name}{i}") for i in range(n)]
        self.k = [f"{name}{i}" for i in range(n)]
        self.i = 0

    def next(self):
        j = self.i % len(self.t)
        self.i += 1
        return self.t[j], self.k[j]


U_POOL, U_CA, U_CG, U_CQ, U_CQR, U_CK, U_CKR, U_IQ, U_IQR, U_IK, U_IKR, U_KR, U_KRR, U_MQ, U_MKV = (
    0, 4, 8, 12, 16, 20, 24, 28, 30, 32, 33, 34, 35, 36, 39)
U_G0, U_G1, U_G2, U_G3, U_CV, U_IW, NU = 41, 49, 57, 65, 73, 77, 78


def rotperm(nh, hd, rot):
    half = rot // 2
    idx = np.arange(nh * hd)
    for h in range(nh):
        for j in range(half):
            idx[h * hd + j] = h * hd + j + half
            idx[h * hd + j + half] = h * hd + j
    return idx


def rope_consts():
    c = np.zeros((128, 8), np.float32)
    for ty, (hd, rot) in enumerate([(64, 16), (32, 8), (32, 32)]):
        half = rot // 2
        inv = (np.float32(THETA) ** (-np.arange(0, rot, 2, dtype=np.float32) / np.float32(rot))).astype(np.float32)
        for p in range(128):
            d = p % hd
            if d < rot:
                c[p, ty] = inv[d % half]
                c[p, 3 + ty] = -1.0 if d < half else 1.0
    return c


def build_p1():
    P = Prog()
    xT = P.dram_in("xT", [1024, H + T], F32)
    W = P.dram_in("W", [1024, NU * 128], F32)
    posd = P.dram_in("pos", [1, T], I32)
    cst = P.dram_in("cst", [128, 8], F32)
    pcorr = P.dram_in("pcorr", [128, 4 * 16], F32)
    poolw = P.dram_in("poolw", [128, 4 * 128], F32)
    pvec = P.dram_in("pvec", [128, 32], F32)
    poolout = P.dram_in("poolout", [512, 1024], F32)
    convw = P.dram_in("convw", [128, 4 * 31], F32)
    convout = P.dram_in("convout", [512, 1024], F32)
    wuqn = P.dram_in("wuqn", [384, 512], F32)
    wuqr = P.dram_in("wuqr", [384, 512], F32)
    wuk = P.dram_in("wuk", [256, 512], F32)
    wuv = P.dram_in("wuv", [256, 512], F32)

    o_ma = P.dram_out("ma", [1024, T], F32)
    o_mb = P.dram_out("mb", [1024, T], F32)
    o_g2 = P.dram_out("g2", [1024, T], BF16)
    o_g3 = P.dram_out("g3", [1024, T], BF16)
    o_qd = P.dram_out("qd", [512, T], BF16)
    o_kd = P.dram_out("kd", [512, T], BF16)
    o_vd = P.dram_out("vd", [T, 520], BF16)
    o_qi = P.dram_out("qi", [256, T], BF16)
    o_ki = P.dram_out("ki", [128, T], BF16)
    o_iw = P.dram_out("iw", [T, 8], F32)
    o_qn = P.dram_out("qn", [512, T], BF16)
    o_qr = P.dram_out("qr", [256, T], BF16)
    o_kn = P.dram_out("kn", [512, T], BF16)
    o_kr = P.dram_out("kr", [128, T], BF16)
    o_vm = P.dram_out("vm", [T, 520], BF16)

    xb = P.sb([128, 8, H + T], BF16, "xb")
    posi_r = Ring(P, 2, [128, TT], I32, "posi")
    posf_r = Ring(P, 2, [128, TT], F32, "posf")
    cst_sb = P.sb([128, 8], F32, "cst_sb")
    pcorr_sb = P.sb([128, 64], F32, "pcorr_sb")
    pvec_sb = P.sb([128, 32], F32, "pvec_sb")
    poolw_sb = P.sb([128, 4, 128], BF16, "poolw_sb")
    poolout_sb = P.sb([128, 4, 1024], BF16, "poolout_sb")
    convw_sb = P.sb([128, 4, 31], F32, "convw_sb")
    convout_sb = P.sb([128, 4, 1024], BF16, "convout_sb")
    wuqn_sb = P.sb([128, 3, 512], BF16, "wuqn_sb")
    wuqr_sb = P.sb([128, 3, 512], BF16, "wuqr_sb")
    wuk_sb = P.sb([128, 2, 512], BF16, "wuk_sb")
    wuv_sb = P.sb([128, 2, 512], BF16, "wuv_sb")
    ones_sb = P.sb([128, 128], F32, "ones_sb")
    wring = [P.sb([128, 8, 512], BF16, f"wr{i}") for i in range(4)]

    for k in range(8):
        P.dma("pool", lambda e, k=k: e.dma_start(out=xb[:, k, :], in_=xT[k * 128:(k + 1) * 128, :]), w=["xb"])
    P.dma("sp", lambda e: e.dma_start(out=cst_sb[:], in_=cst), w=["cst"])
    P.dma("sp", lambda e: e.dma_start(out=pcorr_sb[:], in_=pcorr), w=["pcorr"])
    P.dma("sp", lambda e: e.dma_start(out=pvec_sb[:], in_=pvec), w=["pvec"])
    P.dma("sp", lambda e: e.dma_start(out=convw_sb[:], in_=convw.rearrange("p (c j) -> p c j", j=31)), w=["convw"])
    P.dma("pool", lambda e: e.dma_start(out=poolw_sb[:], in_=poolw.rearrange("p (g d) -> p g d", d=128)), w=["poolw"])
    P.dma("pool", lambda e: e.dma_start(out=poolout_sb[:], in_=poolout.rearrange("(k p) n -> p k n", p=128)), w=["poolout"])
    P.dma("pool", lambda e: e.dma_start(out=convout_sb[:], in_=convout.rearrange("(k p) n -> p k n", p=128)), w=["convout"])
    P.dma("pool", lambda e: e.dma_start(out=wuqn_sb[:], in_=wuqn.rearrange("(k p) n -> p k n", p=128)), w=["wuqn"])
    P.dma("pool", lambda e: e.dma_start(out=wuqr_sb[:], in_=wuqr.rearrange("(k p) n -> p k n", p=128)), w=["wuqr"])
    P.dma("pool", lambda e: e.dma_start(out=wuk_sb[:], in_=wuk.rearrange("(k p) n -> p k n", p=128)), w=["wuk"])
    P.dma("pool", lambda e: e.dma_start(out=wuv_sb[:], in_=wuv.rearrange("(k p) n -> p k n", p=128)), w=["wuv"])
    P.dve(lambda e: e.memset(ones_sb[:], 1.0), w=["ones"])

    PS = [P.ps([128, 512], F32, name=f"psb{i}") for i in range(8)]
    pctr = [0]

    def newps():
        i = pctr[0] % 8
        pctr[0] += 1
        return PS[i], f"ps{i}"

    wctr = [0]

    def loadw(u0):
        i = wctr[0] % 4
        wctr[0] += 1
        wt, wk = wring[i], f"wr{i}"
        nu = min(4, NU - u0)
        P.dma("pool", lambda e: e.dma_start(out=wt[:, :, 0:nu * 128],
                                            in_=W[:, u0 * 128:(u0 + nu) * 128].rearrange("(k p) n -> p k n", p=128)), w=[wk])
        return wt, wk

    def proj(wt, wk, ul, c0, ntok, ncol=128):
        ps, pk = newps()
        for k in range(8):
            P.pe(lambda e, k=k: e.matmul(ps[0:ncol, 0:ntok], lhsT=wt[:, k, ul * 128:ul * 128 + ncol],
                                         rhs=xb[:, k, c0:c0 + ntok], start=(k == 0), stop=(k == 7)),
                 r=[wk, "xb"], w=[pk])
        return ps, pk

    tmpA = Ring(P, 2, [128, 544], F32, "tA")
    tmpB = Ring(P, 2, [128, 544], F32, "tB")
    tmpC = Ring(P, 2, [128, 544], F32, "tC")
    ob32 = Ring(P, 2, [128, 512], F32, "ob32")
    ob16 = Ring(P, 2, [128, 512], BF16, "ob16")
    tabC = [P.sb([128, 512], F32, f"tabC{i}") for i in range(3)]
    tabS = [P.sb([128, 512], F32, f"tabS{i}") for i in range(3)]
    kint = P.sb([128, 512], I32, "kint")
    kflt = P.sb([128, 512], F32, "kflt")

    def make_tables(tt, types):
        posi, pik = posi_r.next()
        posf, pfk = posf_r.next()
        P.dma("sp", lambda e: e.dma_start(out=posi[:], in_=posd[:, tt * TT:(tt + 1) * TT].partition_broadcast(128)), w=[pik])
        P.dve(lambda e: e.tensor_copy(out=posf[:], in_=posi[:]), r=[pik], w=[pfk])
        for ty in types:
            for (tab, nm, ph) in ((tabS[ty], f"tabS{ty}", 0.0), (tabC[ty], f"tabC{ty}", 0.5 * PI)):
                P.dve(lambda e, tab=tab, ty=ty, ph=ph: e.tensor_scalar(
                    out=tab[:], in0=posf[:], scalar1=cst_sb[:, ty:ty + 1], scalar2=ph,
                    op0=ALU.mult, op1=ALU.add), r=[pfk, "cst"], w=[nm])
                P.dve(lambda e, tab=tab: e.tensor_scalar(out=kint[:], in0=tab[:], scalar1=1.0 / (2 * PI), scalar2=None,
                                                         op0=ALU.mult), r=[nm], w=["kint"])
                P.dve(lambda e: e.tensor_copy(out=kflt[:], in_=kint[:]), r=["kint"], w=["kflt"])
                P.dve(lambda e, tab=tab: e.scalar_tensor_tensor(out=tab[:], in0=kflt[:], scalar=-6.28125, in1=tab[:],
                                                                op0=ALU.mult, op1=ALU.add), r=[nm, "kflt"], w=[nm])
                P.dve(lambda e, tab=tab: e.scalar_tensor_tensor(out=tab[:], in0=kflt[:], scalar=-(2 * PI - 6.28125), in1=tab[:],
                                                                op0=ALU.mult, op1=ALU.add), r=[nm, "kflt"], w=[nm])
                P.dve(lambda e, tab=tab: e.tensor_scalar(out=tab[:], in0=tab[:], scalar1=-3.141592, scalar2=3.141592,
                                                         op0=ALU.max, op1=ALU.min), r=[nm], w=[nm])
                P.act(lambda e, tab=tab: e.activation(out=tab[:], in_=tab[:], func=AF.Sin), r=[nm], w=[nm])

    def rope_out(ps1, pk1, ps2, pk2, ty, dst, nrow=128):
        t1, k1 = tmpA.next()
        t2, k2 = tmpB.next()
        ob, ok = ob16.next()
        P.dve(lambda e: e.tensor_tensor(out=t1[:, 0:TT], in0=ps1[:, 0:TT], in1=tabC[ty][:], op=ALU.mult),
              r=[pk1, f"tabC{ty}"], w=[k1])
        P.dve(lambda e: e.scalar_tensor_tensor(out=t2[:, 0:TT], in0=ps2[:, 0:TT], scalar=cst_sb[:, 3 + ty:4 + ty],
                                               in1=tabS[ty][:], op0=ALU.mult, op1=ALU.mult),
              r=[pk2, f"tabS{ty}", "cst"], w=[k2])
        P.pool(lambda e: e.tensor_tensor(out=ob[:], in0=t1[:, 0:TT], in1=t2[:, 0:TT], op=ALU.add), r=[k1, k2], w=[ok])
        P.dma("sp", lambda e: e.dma_start(out=dst, in_=ob[0:nrow, :]), r=[ok])

    wtp, wkp = loadw(U_POOL)
    wg0a, wg0ak = loadw(U_G0)
    wg0b, wg0bk = loadw(U_G0 + 4)
    mixed = [P.sb([128, 4, 512], BF16, f"mixed{i}") for i in range(1)]
    pooled_r = Ring(P, 2, [128, 512], BF16, "pooled")
    WINS = (2, 4, 8, 16)
    for tt in range(NT):
        c0 = H + tt * TT
        mx, mxk = mixed[0], "mixed0"
        for g in range(4):
            psm, pkm = proj(wtp, wkp, g, c0, TT)
            psh, pkh = proj(wtp, wkp, g, c0 - 16, 16)
            U, uk = tmpA.next()
            A, ak = tmpB.next()
            B, bk = tmpC.next()
            P.act(lambda e: e.copy(out=U[:, 16:528], in_=psm[:, 0:512]), r=[pkm], w=[uk])
            P.act(lambda e: e.copy(out=U[:, 0:16], in_=psh[:, 0:16]), r=[pkh], w=[uk])
            src, sk = U, uk
            dsts = [(A, ak), (B, bk)]
            sh = 1
            lo = 1
            for wi in range(g + 1):
                dst, dk = dsts[wi % 2]
                P.dve(lambda e, dst=dst, src=src, lo=lo, sh=sh: e.tensor_tensor(
                    out=dst[:, lo:528], in0=src[:, lo:528], in1=src[:, lo - sh:528 - sh], op=ALU.add), r=[sk], w=[dk])
                src, sk = dst, dk
                sh *= 2
                lo += sh
            other, okk = dsts[(g + 1) % 2]
            P.dve(lambda e: e.tensor_scalar(out=other[:, 16:528], in0=src[:, 16:528], scalar1=1.0 / WINS[g], scalar2=None,
                                            op0=ALU.mult), r=[sk], w=[okk])
            if tt == 0:
                P.dve(lambda e: e.tensor_tensor(out=other[:, 16:32], in0=other[:, 16:32], in1=pcorr_sb[:, g * 16:(g + 1) * 16],
                                                op=ALU.mult), r=[okk, "pcorr"], w=[okk])
            pl, plk = pooled_r.next()
            P.dve(lambda e: e.tensor_tensor(out=pl[:], in0=other[:, 16:528], in1=U[:, 16:528], op=ALU.subtract),
                  r=[okk, uk], w=[plk])
            ps2, pk2 = newps()
            P.pe(lambda e: e.matmul(ps2[:, :], lhsT=poolw_sb[:, g, :], rhs=pl[:], start=True, stop=True),
                 r=["poolw", plk], w=[pk2])
            P.act(lambda e: e.activation(out=mx[:, g, :], in_=ps2[:, :], func=AF.Copy, scale=pvec_sb[:, g:g + 1]),
                  r=[pk2, "pvec"], w=[mxk])
        for m in range(8):
            psy, pky = newps()
            for g in range(4):
                P.pe(lambda e, g=g: e.matmul(psy[:, :], lhsT=poolout_sb[:, g, m * 128:(m + 1) * 128], rhs=mx[:, g, :],
                                             start=(g == 0), stop=(g == 3)), r=["poolout", mxk], w=[pky])
            wt, wk = (wg0a, wg0ak) if m < 4 else (wg0b, wg0bk)
            psg, pkg = proj(wt, wk, m % 4, c0, TT)
            sg, sgk = tmpA.next()
            ob, ok = ob32.next()
            P.act(lambda e: e.activation(out=sg[:, 0:TT], in_=psg[:, :], func=AF.Sigmoid), r=[pkg], w=[sgk])
            P.dve(lambda e: e.tensor_tensor(out=ob[:], in0=psy[:, :], in1=sg[:, 0:TT], op=ALU.mult), r=[pky, sgk], w=[ok])
            P.dma("sp", lambda e, m=m: e.dma_start(out=o_ma[m * 128:(m + 1) * 128, tt * TT:(tt + 1) * TT], in_=ob[:]), r=[ok])

    wca, wcak = loadw(U_CA)
    wcg, wcgk = loadw(U_CG)
    wg1a, wg1ak = loadw(U_G1)
    wg1b, wg1bk = loadw(U_G1 + 4)
    hbuf = Ring(P, 2, [128, 544], F32, "hbuf")
    accs = Ring(P, 2, [128, 512], F32, "acc")
    hc = [P.sb([128, 4, 512], F32, f"hc{i}") for i in range(1)]
    sq = [P.sb([128, 4, 512], F32, f"sq{i}") for i in range(1)]
    hs = [P.sb([128, 4, 512], BF16, f"hs{i}") for i in range(1)]
    P.dve(lambda e: e.memset(ones_sb[:], 1.0), w=["ones"])
    for tt in range(NT):
        c0 = H + tt * TT
        hcc, hck = hc[0], "hc0"
        sqq, sqk = sq[0], "sq0"
        hss, hsk = hs[0], "hs0"
        for c in range(4):
            pa, pak = proj(wca, wcak, c, c0, TT)
            pah, pahk = proj(wca, wcak, c, c0 - 32, 32)
            pg, pgk = proj(wcg, wcgk, c, c0, TT)
            pgh, pghk = proj(wcg, wcgk, c, c0 - 32, 32)
            sg, sgk = tmpA.next()
            hb, hbk = hbuf.next()
            P.act(lambda e: e.activation(out=sg[:, 32:544], in_=pg[:, 0:512], func=AF.Sigmoid), r=[pgk], w=[sgk])
            P.act(lambda e: e.activation(out=sg[:, 0:32], in_=pgh[:, 0:32], func=AF.Sigmoid), r=[pghk], w=[sgk])
            P.dve(lambda e: e.tensor_tensor(out=hb[:, 32:544], in0=pa[:, 0:512], in1=sg[:, 32:544], op=ALU.mult),
                  r=[pak, sgk], w=[hbk])
            P.dve(lambda e: e.tensor_tensor(out=hb[:, 0:32], in0=pah[:, 0:32], in1=sg[:, 0:32], op=ALU.mult),
                  r=[pahk, sgk], w=[hbk])
            a0, a0k = accs.next()
            P.dve(lambda e, c=c: e.tensor_scalar(out=a0[:], in0=hb[:, 2:514], scalar1=convw_sb[:, c, 0:1],
                                                 scalar2=pvec_sb[:, 4 + c:5 + c], op0=ALU.mult, op1=ALU.add),
                  r=[hbk, "convw", "pvec"], w=[a0k])
            cur, curk = a0, a0k
            for j in range(1, 31):
                if j == 30:
                    nxt, nxtk = hcc[:, c, :], hck
                else:
                    nx, nxtk = accs.next()
                    nxt = nx[:]
                P.dve(lambda e, c=c, j=j, cur=cur, nxt=nxt: e.scalar_tensor_tensor(
                    out=nxt, in0=hb[:, 2 + j:514 + j], scalar=convw_sb[:, c, j:j + 1], in1=cur[:] if j == 1 else cur,
                    op0=ALU.mult, op1=ALU.add), r=[hbk, "convw", curk], w=[nxtk])
                cur, curk = nxt, nxtk
            P.act(lambda e, c=c: e.activation(out=sqq[:, c, :], in_=hcc[:, c, :], func=AF.Square), r=[hck], w=[sqk])
        pmean, pmeank = newps()
        pmsq, pmsqk = newps()
        for c in range(4):
            P.pe(lambda e, c=c: e.matmul(pmean[:, :], lhsT=ones_sb[:], rhs=hcc[:, c, :], start=(c == 0), stop=(c == 3)),
                 r=["ones", hck], w=[pmeank])
        for c in range(4):
            P.pe(lambda e, c=c: e.matmul(pmsq[:, :], lhsT=ones_sb[:], rhs=sqq[:, c, :], start=(c == 0), stop=(c == 3)),
                 r=["ones", sqk], w=[pmsqk])
        mean, meank = tmpB.next()
        rstd, rstdk = tmpC.next()
        P.act(lambda e: e.activation(out=mean[:, 0:TT], in_=pmean[:, :], func=AF.Copy, scale=1.0 / 512), r=[pmeank], w=[meank])
        P.dve(lambda e: e.tensor_tensor(out=rstd[:, 0:TT], in0=mean[:, 0:TT], in1=mean[:, 0:TT], op=ALU.mult), r=[meank], w=[rstdk])
        P.dve(lambda e: e.scalar_tensor_tensor(out=rstd[:, 0:TT], in0=pmsq[:, :], scalar=1.0 / 512, in1=rstd[:, 0:TT],
                                               op0=ALU.mult, op1=ALU.subtract), r=[pmsqk, rstdk], w=[rstdk])
        P.dve(lambda e: e.tensor_scalar(out=rstd[:, 0:TT], in0=rstd[:, 0:TT], scalar1=EPS, scalar2=None, op0=ALU.add),
              r=[rstdk], w=[rstdk])
        P.act(lambda e: e.sqrt(out=rstd[:, 0:TT], in_=rstd[:, 0:TT]), r=[rstdk], w=[rstdk])
        P.dve(lambda e: e.reciprocal(out=rstd[:, 0:TT], in_=rstd[:, 0:TT]), r=[rstdk], w=[rstdk])
        for c in range(4):
            d1, d1k = tmpA.next()
            P.dve(lambda e, c=c: e.tensor_tensor(out=d1[:, 0:TT], in0=hcc[:, c, :], in1=mean[:, 0:TT], op=ALU.subtract),
                  r=[hck, meank], w=[d1k])
            P.dve(lambda e: e.tensor_tensor(out=d1[:, 0:TT], in0=d1[:, 0:TT], in1=rstd[:, 0:TT], op=ALU.mult), r=[d1k, rstdk], w=[d1k])
            P.act(lambda e, c=c: e.activation(out=hss[:, c, :], in_=d1[:, 0:TT], func=AF.Silu, scale=pvec_sb[:, 8 + c:9 + c],
                                              bias=pvec_sb[:, 12 + c:13 + c]), r=[d1k, "pvec"], w=[hsk])
        for m in range(8):
            psy, pky = newps()
            for c in range(4):
                P.pe(lambda e, c=c: e.matmul(psy[:, :], lhsT=convout_sb[:, c, m * 128:(m + 1) * 128], rhs=hss[:, c, :],
                                             start=(c == 0), stop=(c == 3)), r=["convout", hsk], w=[pky])
            wt, wk = (wg1a, wg1ak) if m < 4 else (wg1b, wg1bk)
            psg, pkg = proj(wt, wk, m % 4, c0, TT)
            sg, sgk = tmpA.next()
            ob, ok = ob32.next()
            P.act(lambda e: e.activation(out=sg[:, 0:TT], in_=psg[:, :], func=AF.Sigmoid), r=[pkg], w=[sgk])
            P.dve(lambda e: e.tensor_tensor(out=ob[:], in0=psy[:, :], in1=sg[:, 0:TT], op=ALU.mult), r=[pky, sgk], w=[ok])
            P.dma("sp", lambda e, m=m: e.dma_start(out=o_mb[m * 128:(m + 1) * 128, tt * TT:(tt + 1) * TT], in_=ob[:]), r=[ok])

    for (u_main, u_rot, nun, ty, dst) in ((U_CQ, U_CQR, 4, 0, o_qd), (U_CK, U_CKR, 4, 0, o_kd)):
        w1, w1k = loadw(u_main)
        w2, w2k = loadw(u_rot)
        for tt in range(NT):
            c0 = H + tt * TT
            make_tables(tt, [0])
            for u in range(nun):
                p1, p1k = proj(w1, w1k, u, c0, TT)
                p2, p2k = proj(w2, w2k, u, c0, TT)
                rope_out(p1, p1k, p2, p2k, ty, dst[u * 128:(u + 1) * 128, tt * TT:(tt + 1) * TT])
    w1, w1k = loadw(U_IQ)
    w2, w2k = loadw(U_IK)
    for tt in range(NT):
        c0 = H + tt * TT
        make_tables(tt, [1, 2])
        for u in range(2):
            p1, p1k = proj(w1, w1k, u, c0, TT)
            p2, p2k = proj(w1, w1k, 2 + u, c0, TT)
            rope_out(p1, p1k, p2, p2k, 1, o_qi[u * 128:(u + 1) * 128, tt * TT:(tt + 1) * TT])
        p1, p1k = proj(w2, w2k, 0, c0, TT)
        p2, p2k = proj(w2, w2k, 1, c0, TT)
        rope_out(p1, p1k, p2, p2k, 1, o_ki[:, tt * TT:(tt + 1) * TT])
        p1, p1k = proj(w2, w2k, 2, c0, TT)
        p2, p2k = proj(w2, w2k, 3, c0, TT)
        rope_out(p1, p1k, p2, p2k, 2, o_kr[:, tt * TT:(tt + 1) * TT])

    w1, w1k = loadw(U_MQ)
    w2, w2k = loadw(U_MKV + 1 - 1)
    lat = hc
    latn = hs
    vaug = Ring(P, 3, [128, 8, 65], BF16, "vaug")
    for i in range(3):
        P.pool(lambda e, i=i: e.memset(vaug.t[i][:], 1.0), w=[vaug.k[i]])

    def rmsnorm_lat(tt, wt, wk, units, nrm_col0, n_feat):
        c0 = H + tt * TT
        L, lk = lat[0], "hc0"
        LN_, lnk = latn[0], "hs0"
        SQ, sqk = sq[0], "sq0"
        nch = len(units)
        for ci, u in enumerate(units):
            p, pk = proj(wt, wk, u, c0, TT)
            P.act(lambda e, ci=ci: e.copy(out=L[:, ci, :], in_=p[:, :]), r=[pk], w=[lk])
            P.act(lambda e, ci=ci: e.activation(out=SQ[:, ci, :], in_=p[:, :], func=AF.Square), r=[pk], w=[sqk])
        pss, pssk = newps()
        for ci in range(nch):
            P.pe(lambda e, ci=ci: e.matmul(pss[:, :], lhsT=ones_sb[:], rhs=SQ[:, ci, :], start=(ci == 0), stop=(ci == nch - 1)),
                 r=["ones", sqk], w=[pssk])
        rstd, rstdk = tmpC.next()
        P.dve(lambda e: e.tensor_scalar(out=rstd[:, 0:TT], in0=pss[:, :], scalar1=1.0 / n_feat, scalar2=EPS, op0=ALU.mult, op1=ALU.add),
              r=[pssk], w=[rstdk])
        P.act(lambda e: e.sqrt(out=rstd[:, 0:TT], in_=rstd[:, 0:TT]), r=[rstdk], w=[rstdk])
        P.dve(lambda e: e.reciprocal(out=rstd[:, 0:TT], in_=rstd[:, 0:TT]), r=[rstdk], w=[rstdk])
        for ci in range(nch):
            P.dve(lambda e, ci=ci: e.scalar_tensor_tensor(out=LN_[:, ci, :], in0=L[:, ci, :], scalar=pvec_sb[:, nrm_col0 + ci:nrm_col0 + ci + 1],
                                                          in1=rstd[:, 0:TT], op0=ALU.mult, op1=ALU.mult), r=[lk, "pvec", rstdk], w=[lnk])
        return LN_, lnk

    for tt in range(NT):
        make_tables(tt, [2])
        LN_, lnk = rmsnorm_lat(tt, w1, w1k, [0, 1, 2], 16, 384.0)
        for m in range(4):
            ps, pk = newps()
            for k in range(3):
                P.pe(lambda e, k=k: e.matmul(ps[:, :], lhsT=wuqn_sb[:, k, m * 128:(m + 1) * 128], rhs=LN_[:, k, :],
                                             start=(k == 0), stop=(k == 2)), r=["wuqn", lnk], w=[pk])
            ob, ok = ob16.next()
            P.act(lambda e: e.copy(out=ob[:], in_=ps[:, :]), r=[pk], w=[ok])
            P.dma("sp", lambda e, m=m: e.dma_start(out=o_qn[m * 128:(m + 1) * 128, tt * TT:(tt + 1) * TT], in_=ob[:]), r=[ok])
        for m in range(2):
            ps1, pk1 = newps()
            ps2, pk2 = newps()
            for k in range(3):
                P.pe(lambda e, k=k: e.matmul(ps1[:, :], lhsT=wuqr_sb[:, k, m * 128:(m + 1) * 128], rhs=LN_[:, k, :],
                                             start=(k == 0), stop=(k == 2)), r=["wuqr", lnk], w=[pk1])
            for k in range(3):
                P.pe(lambda e, k=k: e.matmul(ps2[:, :], lhsT=wuqr_sb[:, k, 256 + m * 128:256 + (m + 1) * 128], rhs=LN_[:, k, :],
                                             start=(k == 0), stop=(k == 2)), r=["wuqr", lnk], w=[pk2])
            rope_out(ps1, pk1, ps2, pk2, 2, o_qr[m * 128:(m + 1) * 128, tt * TT:(tt + 1) * TT])
    for tt in range(NT):
        LN_, lnk = rmsnorm_lat(tt, w2, w2k, [0, 1], 19, 256.0)
        for m in range(4):
            ps, pk = newps()
            for k in range(2):
                P.pe(lambda e, k=k: e.matmul(ps[:, :], lhsT=wuk_sb[:, k, m * 128:(m + 1) * 128], rhs=LN_[:, k, :],
                                             start=(k == 0), stop=(k == 1)), r=["wuk", lnk], w=[pk])
            ob, ok = ob16.next()
            P.act(lambda e: e.copy(out=ob[:], in_=ps[:, :]), r=[pk], w=[ok])
            P.dma("sp", lambda e, m=m: e.dma_start(out=o_kn[m * 128:(m + 1) * 128, tt * TT:(tt + 1) * TT], in_=ob[:]), r=[ok])
        for s in range(4):
            ps, pk = newps()
            for k in range(2):
                P.pe(lambda e, k=k: e.matmul(ps[:, :], lhsT=LN_[:, k, s * 128:(s + 1) * 128], rhs=wuv_sb[:, k, :],
                                             start=(k == 0), stop=(k == 1)), r=["wuv", lnk], w=[pk])
            va, vak = vaug.next()
            P.act(lambda e: e.copy(out=va[:, :, 0:64], in_=ps[:, :].rearrange("p (h d) -> p h d", d=64)), r=[pk], w=[vak])
            r0 = tt * TT + s * 128
            P.dma("sp", lambda e, r0=r0: e.dma_start(out=o_vm[r0:r0 + 128, :], in_=va[:].rearrange("p h d -> p (h d)")), r=[vak])

    for (u0, dst) in ((U_G2, o_g2), (U_G3, o_g3)):
        wa, wak = loadw(u0)
        wb, wbk = loadw(u0 + 4)
        for tt in range(NT):
            c0 = H + tt * TT
            for m in range(8):
                wt, wk = (wa, wak) if m < 4 else (wb, wbk)
                psg, pkg = proj(wt, wk, m % 4, c0, TT)
                ob, ok = ob16.next()
                P.act(lambda e: e.activation(out=ob[:], in_=psg[:, :], func=AF.Sigmoid), r=[pkg], w=[ok])
                P.dma("sp", lambda e, m=m: e.dma_start(out=dst[m * 128:(m + 1) * 128, tt * TT:(tt + 1) * TT], in_=ob[:]), r=[ok])

    wv, wvk = loadw(U_CV)
    ww, wwk = loadw(U_IW)
    iwb = Ring(P, 3, [128, 8], F32, "iwb")
    for s in range(T // 128):
        c0 = H + s * 128
        ps, pk = newps()
        for k in range(8):
            P.pe(lambda e, k=k: e.matmul(ps[:, :], lhsT=xb[:, k, c0:c0 + 128], rhs=wv[:, k, :], start=(k == 0), stop=(k == 7)),
                 r=["xb", wvk], w=[pk])
        va, vak = vaug.next()
        P.act(lambda e: e.copy(out=va[:, :, 0:64], in_=ps[:, :].rearrange("p (h d) -> p h d", d=64)), r=[pk], w=[vak])
        P.dma("sp", lambda e: e.dma_start(out=o_vd[s * 128:(s + 1) * 128, :], in_=va[:].rearrange("p h d -> p (h d)")), r=[vak])
        ps2, pk2 = newps()
        for k in range(8):
            P.pe(lambda e, k=k: e.matmul(ps2[:, 0:8], lhsT=xb[:, k, c0:c0 + 128], rhs=ww[:, k, 0:8], start=(k == 0), stop=(k == 7)),
                 r=["xb", wwk], w=[pk2])
        ib, ibk = iwb.next()
        P.act(lambda e: e.copy(out=ib[:], in_=ps2[:, 0:8]), r=[pk2], w=[ibk])
        P.dma("sp", lambda e: e.dma_start(out=o_iw[s * 128:(s + 1) * 128, :], in_=ib[:]), r=[ibk])
    nc = P.finish()
    return nc, P.stats


def prep_w1(inp, l):
    w = inp["w_in"][l]
    cols = []
    cols.append(np.arange(O_POOL, O_POOL + 512))
    cols.append(np.arange(O_CA, O_CA + 512))
    cols.append(np.arange(O_CG, O_CG + 512))
    rp = rotperm(8, 64, 16)
    cols.append(O_CQ + np.arange(512)); cols.append(O_CQ + rp)
    cols.append(O_CK + np.arange(512)); cols.append(O_CK + rp)
    rpi = rotperm(8, 32, 8)
    cols.append(O_IQ + np.arange(256)); cols.append(O_IQ + rpi)
    r1 = rotperm(1, 32, 8)
    cols.append(np.concatenate([O_IK + np.arange(32)] * 4)); cols.append(np.concatenate([O_IK + r1] * 4))
    r2 = rotperm(1, 32, 32)
    cols.append(np.concatenate([O_MKR + np.arange(32)] * 4)); cols.append(np.concatenate([O_MKR + r2] * 4))
    cols.append(O_MQ + np.arange(384)); cols.append(O_MKV + np.arange(256))
    cols.append(O_G + np.arange(4096))
    cols.append(O_CV + np.arange(512))
    cols.append(np.concatenate([O_IW + np.arange(8)] * 16))
    idx = np.concatenate(cols)
    assert idx.shape[0] == NU * 128, idx.shape
    return np.ascontiguousarray(w[:, idx])


def fm(v, n):
    return np.ascontiguousarray(v.reshape(n, 128).T)


def prep_p1(inp, l, xs):
    W1 = prep_w1(inp, l)
    pvec = np.zeros((128, 32), np.float32)
    pvec[:, 0:4] = fm(inp["pool_scale"][l], 4)
    pvec[:, 4:8] = fm(inp["conv_b"][l], 4)
    pvec[:, 8:12] = fm(inp["conv_ln_g"][l], 4)
    pvec[:, 12:16] = fm(inp["conv_ln_b"][l], 4)
    pvec[:, 16:19] = fm(inp["mla_q_norm"][l], 3)
    pvec[:, 19:21] = fm(inp["mla_kv_norm"][l], 2)
    poolw = np.ascontiguousarray(inp["pool_w"][l].transpose(1, 0, 2).reshape(128, 512))
    convw = np.ascontiguousarray(inp["conv_w"][l].T.reshape(4, 128, 31).transpose(1, 0, 2).reshape(128, 124))
    wuq = inp["mla_wuq"][l]
    hn = np.concatenate([96 * h + np.arange(64) for h in range(8)])
    hr = np.concatenate([96 * h + 64 + np.arange(32) for h in range(8)])
    rp = rotperm(8, 32, 32)
    wuqn = np.ascontiguousarray(wuq[:, hn])
    wuqr = np.ascontiguousarray(np.concatenate([wuq[:, hr], wuq[:, hr[rp]]], axis=1))
    cst = rope_consts()
    maps = []
    for c in range(8):
        b, half = c // 2, c % 2
        x = inp["x_cur"][b]
        s0 = half * T
        xh = np.zeros((H + T, 1024), np.float32)
        xh[H:] = x[s0:s0 + T]
        if half:
            xh[:H] = x[s0 - H:s0]
        pc = np.ones((128, 4, 16), np.float32)
        if half == 0:
            for g, wv in enumerate((2, 4, 8, 16)):
                pc[:, g, :] = wv / np.minimum(np.arange(16) + 1, wv).astype(np.float32)
        maps.append(dict(xT=np.ascontiguousarray(xh.T), W=W1, pos=inp["positions"][b:b + 1, s0:s0 + T].astype(np.int32),
                         cst=cst, pcorr=pc.reshape(128, 64), poolw=poolw, pvec=pvec, poolout=inp["pool_out"][l],
                         convw=convw, convout=inp["conv_out"][l], wuqn=wuqn, wuqr=wuqr,
                         wuk=inp["mla_wuk"][l], wuv=inp["mla_wuv"][l]))
    return maps


S = 8192
NSLOT = 4
MLA_SCALE = 96 ** -0.5


def slot_tiles(j):
    return 8 * j + 8, 64 - 8 * j


def core_groups(half):
    return [((2 * j + half), (15 - 2 * j - half)) for j in range(NSLOT)]


def build_mla():
    P = Prog()
    qn = P.dram_in("qn", [512, 4096], BF16)
    qr = P.dram_in("qr", [512, 4096], BF16)
    kn = P.dram_in("kn", [512, S], BF16)
    kr = P.dram_in("kr", [128, S], BF16)
    vm = P.dram_in("vm", [S, 520], BF16)
    msk = P.dram_in("msk", [2 * 8 * 128, 512], BF16)
    idn = P.dram_in("idn", [128, 128], BF16)
    o_o = P.dram_out("o", [512, 4096], BF16)

    kn_sb = P.sb([128, 4, S], BF16, "kn_sb")
    kr_sb = P.sb([128, S], BF16, "kr_sb")
    vm_sb = P.sb([128, 64, 520], BF16, "vm_sb")
    msk_sb = P.sb([128, 16, 512], BF16, "msk_sb")
    idn_sb = P.sb([128, 128], BF16, "idn_sb")
    ones_sb = P.sb([128, 64], F32, "ones_sb")
    qn_r = Ring(P, 2, [128, 4, 512], BF16, "qn_r")
    qr_r = Ring(P, 2, [128, 4, 512], BF16, "qr_r")
    pT_r = Ring(P, 3, [128, 512], BF16, "pT")
    osb_r = Ring(P, 2, [128, 512], F32, "osb")
    rcp_r = Ring(P, 2, [64, 512], F32, "rcp")
    on_r = Ring(P, 2, [64, 512], BF16, "on")

    for c in range(4):
        P.dma("sp" if c % 2 else "act", lambda e, c=c: e.dma_start(out=kn_sb[:, c, :], in_=kn[c * 128:(c + 1) * 128, :]), w=["kn"])
    P.dma("sp", lambda e: e.dma_start(out=kr_sb[:], in_=kr), w=["kr"])
    for q4 in range(4):
        P.dma("pool", lambda e, q4=q4: e.dma_start(out=vm_sb[:, q4 * 16:(q4 + 1) * 16, :],
                                                   in_=vm[q4 * 2048:(q4 + 1) * 2048, :].rearrange("(t p) n -> p t n", p=128)), w=["vm"])
    P.dma("sp", lambda e: e.dma_start(out=msk_sb[:], in_=msk.rearrange("(t p) n -> p t n", p=128)), w=["msk"])
    P.dma("sp", lambda e: e.dma_start(out=idn_sb[:], in_=idn), w=["idn"])
    P.dve(lambda e: e.memset(ones_sb[:], 1.0), w=["ones"])

    PSQ = [P.ps([128, 512], F32, name=f"psq{i}") for i in range(4)]
    PSO = [P.ps([128, 512], F32, name=f"pso{i}") for i in range(2)]
    PSB = [P.ps([128, 512], F32, name=f"psbb{i}") for i in range(2)]
    ctr = dict(q=0, o=0, b=0)

    for j in range(NSLOT):
        for part, ntiles in enumerate(slot_tiles(j)):
            col0 = (2 * j + part) * 512
            qnt, qnk = qn_r.next()
            qrt, qrk = qr_r.next()
            P.dma("sp", lambda e: e.dma_start(out=qnt[:], in_=qn[:, col0:col0 + 512].rearrange("(c p) n -> p c n", p=128)), w=[qnk])
            P.dma("sp", lambda e: e.dma_start(out=qrt[:], in_=qr[:, col0:col0 + 512].rearrange("(c p) n -> p c n", p=128)), w=[qrk])
            for h in range(8):
                ch, b0 = h // 2, 64 * (h % 2)
                rc, rb = h // 2, 32 * (h % 2)
                po = PSO[ctr["o"] % 2]
                pok = f"pso{ctr['o'] % 2}"
                ctr["o"] += 1
                for kt in range(ntiles):
                    ps = PSQ[ctr["q"] % 4]
                    psk = f"psq{ctr['q'] % 4}"
                    ctr["q"] += 1
                    mi = kt - (ntiles - 8)
                    ks = slice(kt * 128, (kt + 1) * 128)
                    P.pe(lambda e: e.matmul(ps[:, :], lhsT=kn_sb[b0:b0 + 64, ch, ks], rhs=qnt[b0:b0 + 64, ch, :], start=True, stop=False),
                         r=["kn", qnk], w=[psk])
                    P.pe(lambda e: e.matmul(ps[:, :], lhsT=kr_sb[rb:rb + 32, ks], rhs=qrt[rb:rb + 32, rc, :], start=False, stop=(mi < 0)),
                         r=["kr", qrk], w=[psk])
                    if mi >= 0:
                        P.pe(lambda e: e.matmul(ps[:, :], lhsT=idn_sb[:], rhs=msk_sb[:, part * 8 + mi, :], start=False, stop=True),
                             r=["idn", "msk"], w=[psk])
                    pT, pTk = pT_r.next()
                    P.act(lambda e: e.activation(out=pT[:], in_=ps[:, :], func=AF.Exp, scale=MLA_SCALE), r=[psk], w=[pTk])
                    P.pe(lambda e: e.matmul(po[0:65, :], lhsT=vm_sb[:, kt, h * 65:(h + 1) * 65], rhs=pT[:], start=(kt == 0), stop=(kt == ntiles - 1)),
                         r=["vm", pTk], w=[pok])
                osb, osk = osb_r.next()
                P.act(lambda e: e.copy(out=osb[0:65, :], in_=po[0:65, :]), r=[pok], w=[osk])
                pb = PSB[ctr["b"] % 2]
                pbk = f"psbb{ctr['b'] % 2}"
                ctr["b"] += 1
                P.pe(lambda e: e.matmul(pb[0:64, :], lhsT=ones_sb[64:65, 0:64], rhs=osb[64:65, :], start=True, stop=True),
                     r=["ones", osk], w=[pbk])
                rcp, rck = rcp_r.next()
                on, onk = on_r.next()
                P.dve(lambda e: e.reciprocal(out=rcp[:], in_=pb[0:64, :]), r=[pbk], w=[rck])
                P.dve(lambda e: e.tensor_tensor(out=on[:], in0=osb[0:64, :], in1=rcp[:], op=ALU.mult), r=[osk, rck], w=[onk])
                P.dma("sp", lambda e: e.dma_start(out=o_o[h * 64:(h + 1) * 64, col0:col0 + 512], in_=on[:]), r=[onk])
    nc = P.finish()
    return nc, P.stats


def make_masks(half):
    NEG = -30000.0
    kk = np.arange(128)[:, None]
    qq = np.arange(512)[None, :]
    D = [np.where(j * 128 + kk <= qq, 0.0, NEG).astype(np.float32) for j in range(4)]
    Z = np.zeros((128, 512), np.float32)
    F = np.full((128, 512), NEG, np.float32)
    dz = [D[0], D[1], D[2], D[3], F, F, F, F]
    zd = [Z, Z, Z, Z, D[0], D[1], D[2], D[3]]
    lo, hi = (dz, zd) if half == 0 else (zd, dz)
    return np.stack(lo + hi).reshape(16 * 128, 512).astype(NPBF)


def qcols(half):
    cols = []
    for (lo, hi) in core_groups(half):
        cols.append(np.arange(lo * 512, (lo + 1) * 512))
        cols.append(np.arange(hi * 512, (hi + 1) * 512))
    return np.concatenate(cols)


def relayout_qr(qr):
    out = np.zeros((512, qr.shape[1]), qr.dtype)
    for h in range(8):
        r0 = (h // 2) * 128 + (h % 2) * 32
        out[r0:r0 + 32] = qr[h * 32:(h + 1) * 32]
    return out


DSA_SCALE = 64 ** -0.5
NIT = 26
NEG_S = -1.0e9


def build_dsa():
    P = Prog()
    qd = P.dram_in("qd", [512, 4096], BF16)
    kd = P.dram_in("kd", [512, S], BF16)
    vd = P.dram_in("vd", [S, 520], BF16)
    qi = P.dram_in("qi", [512, 4096], BF16)
    ki = P.dram_in("ki", [128, S], BF16)
    iw = P.dram_in("iw", [4096, 8], F32)
    cmsk = P.dram_in("cmsk", [2 * 4 * 128, 1024], F32)
    idn = P.dram_in("idn", [128, 128], BF16)
    o_o = P.dram_out("o", [512, 4096], BF16)
    mscr = [[P.dram_tmp(f"mscr{j}_{p}", [512, slot_tiles(j)[p] * 128], BF16) for p in range(2)] for j in range(NSLOT)]

    PSQ = [P.ps([128, 512], F32, name=f"psq{i}") for i in range(4)]
    PSO = [P.ps([128, 512], F32, name=f"pso{i}") for i in range(2)]
    PSB = [P.ps([128, 512], F32, name=f"psbb{i}") for i in range(2)]
    ctr = dict(q=0, o=0, b=0)

    st1 = ExitStack()
    def sb1(shape, dt, name):
        return st1.enter_context(P.nc.sbuf_tensor(name, list(shape), dt))
    ki_sb = sb1([128, S], BF16, "ki_sb")
    qi_sb = [sb1([128, 4, 512], BF16, f"qi_sb{i}") for i in range(2)]
    iw_sb = [sb1([128, 4, 8], F32, f"iw_sb{i}") for i in range(2)]
    cm_sb = sb1([128, 8, 1024], F32, "cm_sb")
    sc = [sb1([128, S], F32, f"sc{i}") for i in range(2)]
    junk = sb1([128, S], BF16, "junk")
    mbo = [sb1([128, S], BF16, f"mbo{i}") for i in range(2)]
    rl = [sb1([128, 512], F32, f"rl{i}") for i in range(3)]
    sm = [sb1([128, 16], F32, f"sm{i}") for i in range(2)]
    P.dma("sp", lambda e: e.dma_start(out=ki_sb[:], in_=ki), w=["ki"])
    P.dma("act", lambda e: e.dma_start(out=cm_sb[:], in_=cmsk.rearrange("(t p) n -> p t n", p=128)), w=["cm"])
    gi = 0
    rli = 0
    for j in range(NSLOT):
        for part, ntiles in enumerate(slot_tiles(j)):
            col0 = (2 * j + part) * 512
            L = ntiles * 128
            qit, qik = qi_sb[gi % 2], f"qi{gi % 2}"
            iwt, iwk = iw_sb[gi % 2], f"iw{gi % 2}"
            gi += 1
            P.dma("sp", lambda e: e.dma_start(out=qit[:], in_=qi[:, col0:col0 + 512].rearrange("(c p) n -> p c n", p=128)), w=[qik])
            P.dma("sp", lambda e: e.dma_start(out=iwt[:], in_=iw[col0:col0 + 512, :].rearrange("(r p) h -> p r h", p=128)), w=[iwk])
            for r in range(4):
                bi = (gi * 4 + r) % 2
                s_, sk = sc[bi], f"sc{bi}"
                m_, mk = mbo[bi], f"mbo{bi}"
                z_, zk = sm[bi], f"sm{bi}"
                for kc in range(L // 512):
                    for h in range(8):
                        ps = PSQ[ctr["q"] % 4]
                        psk = f"psq{ctr['q'] % 4}"
                        ctr["q"] += 1
                        rc, rb = h // 2, 32 * (h % 2)
                        P.pe(lambda e: e.matmul(ps[:, :], lhsT=qit[rb:rb + 32, rc, r * 128:(r + 1) * 128],
                                                rhs=ki_sb[rb:rb + 32, kc * 512:(kc + 1) * 512], start=True, stop=True),
                             r=[qik, "ki"], w=[psk])
                        rt, rk = rl[rli % 3], f"rl{rli % 3}"
                        rli += 1
                        P.act(lambda e: e.activation(out=rt[:], in_=ps[:, :], func=AF.Relu), r=[psk], w=[rk])
                        if h == 0:
                            P.dve(lambda e: e.tensor_scalar(out=s_[:, kc * 512:(kc + 1) * 512], in0=rt[:], scalar1=iwt[:, r, 0:1],
                                                            scalar2=None, op0=ALU.mult), r=[rk, iwk], w=[sk])
                        else:
                            P.dve(lambda e: e.scalar_tensor_tensor(out=s_[:, kc * 512:(kc + 1) * 512], in0=rt[:], scalar=iwt[:, r, h:h + 1],
                                                                   in1=s_[:, kc * 512:(kc + 1) * 512], op0=ALU.mult, op1=ALU.add),
                                  r=[rk, iwk, sk], w=[sk])
                P.dve(lambda e: e.tensor_reduce(out=z_[:, 0:1], in_=s_[:, 0:L], axis=AX.X, op=ALU.max), r=[sk], w=[zk])
                P.dve(lambda e: e.tensor_reduce(out=z_[:, 7:8], in_=s_[:, 0:L], axis=AX.X, op=ALU.min), r=[sk], w=[zk])
                P.dve(lambda e: e.scalar_tensor_tensor(out=z_[:, 0:1], in0=z_[:, 7:8], scalar=-1.0, in1=z_[:, 0:1], op0=ALU.mult, op1=ALU.max),
                      r=[zk], w=[zk])
                P.dve(lambda e: e.tensor_scalar(out=z_[:, 0:1], in0=z_[:, 0:1], scalar1=1.0, scalar2=None, op0=ALU.add), r=[zk], w=[zk])
                P.dve(lambda e: e.tensor_scalar(out=z_[:, 1:2], in0=z_[:, 0:1], scalar1=2.0, scalar2=None, op0=ALU.mult), r=[zk], w=[zk])
                P.dve(lambda e: e.tensor_scalar(out=z_[:, 2:3], in0=z_[:, 0:1], scalar1=-1.0, scalar2=None, op0=ALU.mult), r=[zk], w=[zk])
                P.dve(lambda e: e.tensor_tensor(out=s_[:, L - 1024:L], in0=s_[:, L - 1024:L], in1=cm_sb[:, part * 4 + r, :], op=ALU.add),
                      r=[sk, "cm"], w=[sk])
                lo_c, lo_n = 2, 3
                for it in range(1, NIT + 1):
                    f = 2.0 ** (-it)
                    P.dve(lambda e: e.scalar_tensor_tensor(out=z_[:, 4:5], in0=z_[:, 1:2], scalar=f, in1=z_[:, lo_c:lo_c + 1],
                                                           op0=ALU.mult, op1=ALU.add), r=[zk], w=[zk])
                    P.dve(lambda e: e.memset(z_[:, 5:6], 0.0), w=[zk])
                    P.dve(lambda e: e.tensor_scalar(out=junk[:, 0:L], in0=s_[:, 0:L], scalar1=z_[:, 4:5], scalar2=0.0,
                                                    op0=ALU.is_ge, op1=ALU.add, accum_out=z_[:, 5:6]), r=[sk, zk], w=["junk", zk])
                    P.dve(lambda e: e.tensor_scalar(out=z_[:, 6:7], in0=z_[:, 5:6], scalar1=255.5, scalar2=f, op0=ALU.is_ge, op1=ALU.mult),
                          r=[zk], w=[zk])
                    P.dve(lambda e: e.scalar_tensor_tensor(out=z_[:, lo_n:lo_n + 1], in0=z_[:, 6:7], scalar=z_[:, 1:2], in1=z_[:, lo_c:lo_c + 1],
                                                           op0=ALU.mult, op1=ALU.add), r=[zk], w=[zk])
                    lo_c, lo_n = lo_n, lo_c
                P.dve(lambda e: e.tensor_scalar(out=m_[:, 0:L], in0=s_[:, 0:L], scalar1=z_[:, lo_c:lo_c + 1], scalar2=-30000.0,
                                                op0=ALU.is_lt, op1=ALU.mult), r=[sk, zk], w=[mk])
                P.dma("sp", lambda e: e.dma_start(out=mscr[j][part][r * 128:(r + 1) * 128, :], in_=m_[:, 0:L]), r=[mk], w=[f"mscr{j}_{part}"])
    P.barrier()
    st1.close()

    kd_sb = P.sb([128, 4, S], BF16, "kd_sb")
    vd_sb = P.sb([128, 64, 520], BF16, "vd_sb")
    idn_sb = P.sb([128, 128], BF16, "idn_sb")
    ones_sb = P.sb([128, 64], F32, "ones_sb")
    mb_sb = P.sb([128, 4, S], BF16, "mb_sb")
    qd_r = Ring(P, 1, [128, 4, 512], BF16, "qd_r")
    pT_r = Ring(P, 3, [128, 512], BF16, "pT")
    osb_r = Ring(P, 1, [128, 512], F32, "osb")
    rcp_r = Ring(P, 1, [64, 512], F32, "rcp")
    on_r = Ring(P, 2, [64, 512], BF16, "on")
    for c in range(4):
        P.dma("sp" if c % 2 else "act", lambda e, c=c: e.dma_start(out=kd_sb[:, c, :], in_=kd[c * 128:(c + 1) * 128, :]), w=["kd"])
    for q4 in range(4):
        P.dma("pool", lambda e, q4=q4: e.dma_start(out=vd_sb[:, q4 * 16:(q4 + 1) * 16, :],
                                                   in_=vd[q4 * 2048:(q4 + 1) * 2048, :].rearrange("(t p) n -> p t n", p=128)), w=["vd"])
    P.dma("sp", lambda e: e.dma_start(out=idn_sb[:], in_=idn), w=["idn"])
    P.dve(lambda e: e.memset(ones_sb[:], 1.0), w=["ones"])
    for j in range(NSLOT):
        for part, ntiles in enumerate(slot_tiles(j)):
            col0 = (2 * j + part) * 512
            L = ntiles * 128
            qdt, qdk = qd_r.next()
            P.dma("sp", lambda e: e.dma_start(out=qdt[:], in_=qd[:, col0:col0 + 512].rearrange("(c p) n -> p c n", p=128)), w=[qdk])
            P.dma("act", lambda e: e.dma_start(out=mb_sb[:, :, 0:L], in_=mscr[j][part].rearrange("(r p) n -> p r n", p=128)),
                  r=[f"mscr{j}_{part}"], w=["mb"])
            for h in range(8):
                ch, b0 = h // 2, 64 * (h % 2)
                po = PSO[ctr["o"] % 2]
                pok = f"pso{ctr['o'] % 2}"
                ctr["o"] += 1
                for kt in range(ntiles):
                    ps = PSQ[ctr["q"] % 4]
                    psk = f"psq{ctr['q'] % 4}"
                    ctr["q"] += 1
                    ks = slice(kt * 128, (kt + 1) * 128)
                    P.pe(lambda e: e.matmul(ps[:, :], lhsT=kd_sb[b0:b0 + 64, ch, ks], rhs=qdt[b0:b0 + 64, ch, :], start=True, stop=False),
                         r=["kd", qdk], w=[psk])
                    for r in range(4):
                        P.pe(lambda e, r=r: e.matmul(ps[:, r * 128:(r + 1) * 128], lhsT=mb_sb[:, r, ks], rhs=idn_sb[:], start=False, stop=(r == 3)),
                             r=["mb", "idn"], w=[psk])
                    pT, pTk = pT_r.next()
                    P.act(lambda e: e.activation(out=pT[:], in_=ps[:, :], func=AF.Exp, scale=DSA_SCALE), r=[psk], w=[pTk])
                    P.pe(lambda e: e.matmul(po[0:65, :], lhsT=vd_sb[:, kt, h * 65:(h + 1) * 65], rhs=pT[:], start=(kt == 0), stop=(kt == ntiles - 1)),
                         r=["vd", pTk], w=[pok])
                osb, osk = osb_r.next()
                P.act(lambda e: e.copy(out=osb[0:65, :], in_=po[0:65, :]), r=[pok], w=[osk])
                pb = PSB[ctr["b"] % 2]
                pbk = f"psbb{ctr['b'] % 2}"
                ctr["b"] += 1
                P.pe(lambda e: e.matmul(pb[0:64, :], lhsT=ones_sb[64:65, 0:64], rhs=osb[64:65, :], start=True, stop=True),
                     r=["ones", osk], w=[pbk])
                rcp, rck = rcp_r.next()
                on, onk = on_r.next()
                P.dve(lambda e: e.reciprocal(out=rcp[:], in_=pb[0:64, :]), r=[pbk], w=[rck])
                P.dve(lambda e: e.tensor_tensor(out=on[:], in0=osb[0:64, :], in1=rcp[:], op=ALU.mult), r=[osk, rck], w=[onk])
                P.dma("sp", lambda e: e.dma_start(out=o_o[h * 64:(h + 1) * 64, col0:col0 + 512], in_=on[:]), r=[onk])
    nc = P.finish()
    return nc, P.stats


def make_cmask(half):
    qq = np.arange(512)[:, None]
    out = np.zeros((2, 4, 128, 1024), np.float32)
    for part in range(2):
        dz = (part == 0) == (half == 0)
        kk = np.arange(1024)[None, :]
        if dz:
            keyrel = kk
        else:
            keyrel = kk - 512
        m = np.where(keyrel <= qq, 0.0, NEG_S).astype(np.float32)
        out[part] = m.reshape(4, 128, 1024)
    return out.reshape(8 * 128, 1024)


NT3 = 16384
NEXP = 32
STOP = 0
ALPHA = 4.0 ** 0.25
EPS3 = 1e-5
SWA = 1.702
LIM = 7.0


def build_p3():
    P = Prog()
    ma = P.dram_in("ma", [1024, NT3], F32)
    mb = P.dram_in("mb", [1024, NT3], F32)
    g2 = P.dram_in("g2", [1024, NT3], BF16)
    g3 = P.dram_in("g3", [1024, NT3], BF16)
    oc = P.dram_in("oc", [512, NT3], BF16)
    od = P.dram_in("od", [512, NT3], BF16)
    x = P.dram_in("x", [NT3, 1024], F32)
    dsaout = P.dram_in("dsaout", [512, 1024], F32)
    mlaout = P.dram_in("mlaout", [512, 1024], F32)
    wo = P.dram_in("wo", [1024, 1024], F32)
    lnv = P.dram_in("lnv", [1, 4096], F32)
    rw = P.dram_in("rw", [1024, 32], F32)
    rb = P.dram_in("rb", [1, 32], F32)
    wgu = P.dram_in("wgu", [NEXP * 1024, 2048], F32)
    bgu = P.dram_in("bgu", [NEXP * 128, 16], F32)
    wd = P.dram_in("wd", [NEXP * 1024, 1024], F32)
    bd = P.dram_in("bd", [NEXP, 1024], F32)
    idn = P.dram_in("idn", [128, 128], F32)
    o_x2 = P.dram_out("x2", [NT3, 1024], F32)
    x1_d = P.dram_tmp("x1_d", [NT3, 1024], F32)
    x1T_d = P.dram_tmp("x1T_d", [1024, NT3], BF16)
    G_d = P.dram_tmp("G_d", [NT3, 32], F32)

    PS = [P.ps([128, 512], F32, name=f"psb{i}") for i in range(8)]
    pctr = [0]

    def chk(n):
        if STOP == n:
            P.dead = True

    def newps():
        i = pctr[0] % 8
        pctr[0] += 1
        return PS[i], f"ps{i}"

    def layernorm(u, uk, gb, gbk, gi, out, outk, tmp):
        st, stk = tmp["st"]
        c, ck = tmp["c"]
        jk, jkk = tmp["junk"]
        P.dve(lambda e: e.tensor_reduce(out=st[:, 0:1], in_=u, axis=AX.X, op=ALU.add), r=[uk], w=[stk])
        P.dve(lambda e: e.tensor_scalar(out=st[:, 1:2], in0=st[:, 0:1], scalar1=-1.0 / 1024, scalar2=None, op0=ALU.mult), r=[stk], w=[stk])
        P.dve(lambda e: e.tensor_scalar(out=c[:], in0=u, scalar1=st[:, 1:2], scalar2=None, op0=ALU.add), r=[uk, stk], w=[ck])
        P.act(lambda e: e.activation(out=jk[:], in_=c[:], func=AF.Square), r=[ck], w=[jkk])
        P.dve(lambda e: e.tensor_reduce(out=st[:, 2:3], in_=jk[:], axis=AX.X, op=ALU.add), r=[jkk], w=[stk])
        P.dve(lambda e: e.tensor_scalar(out=st[:, 3:4], in0=st[:, 2:3], scalar1=1.0 / 1024, scalar2=EPS3, op0=ALU.mult, op1=ALU.add),
              r=[stk], w=[stk])
        P.act(lambda e: e.sqrt(out=st[:, 4:5], in_=st[:, 3:4]), r=[stk], w=[stk])
        P.dve(lambda e: e.reciprocal(out=st[:, 5:6], in_=st[:, 4:5]), r=[stk], w=[stk])
        P.dve(lambda e: e.scalar_tensor_tensor(out=jk[:], in0=c[:], scalar=st[:, 5:6], in1=gb[:, gi * 1024:(gi + 1) * 1024], op0=ALU.mult, op1=ALU.mult),
              r=[ck, stk, gbk, jkk], w=[jkk])
        P.pool(lambda e: e.tensor_tensor(out=out, in0=jk[:], in1=gb[:, (gi + 1) * 1024:(gi + 2) * 1024], op=ALU.add), r=[jkk, gbk], w=[outk])

    gb_sb = P.sb([128, 4096], F32, "gb_sb")
    P.dma("sp", lambda e: e.dma_start(out=gb_sb[:], in_=lnv.partition_broadcast(128)), w=["gb"])
    st_r = Ring(P, 2, [128, 8], F32, "st")
    c_r = Ring(P, 1, [128, 1024], F32, "cc")
    jk_r = Ring(P, 1, [128, 1024], F32, "jk")
    u_r = Ring(P, 2, [128, 1024], F32, "u")
    x1s_r = Ring(P, 2, [128, 1024], F32, "x1s")

    def lntmp():
        return dict(st=st_r.next(), c=c_r.next(), junk=jk_r.next())

    stA = ExitStack()

    def sbA(shape, dt, name):
        return stA.enter_context(P.nc.sbuf_tensor(name, list(shape), dt))

    dsaout_sb = sbA([128, 4, 1024], BF16, "dsaout_sb")
    mlaout_sb = sbA([128, 4, 1024], BF16, "mlaout_sb")
    wo_sb = sbA([128, 8, 1024], BF16, "wo_sb")
    rw_sb = sbA([128, 8, 32], F32, "rw_sb")
    rb_sb = sbA([128, 32], F32, "rb_sb")
    idn_sb = sbA([128, 128], F32, "idn_sb")
    ma_sb = sbA([128, 8, 512], F32, "ma_sb")
    mb_sb = sbA([128, 8, 512], F32, "mb_sb")
    g2_sb = sbA([128, 8, 512], BF16, "g2_sb")
    g3_sb = sbA([128, 8, 512], BF16, "g3_sb")
    oc_sb = sbA([128, 4, 512], BF16, "oc_sb")
    od_sb = sbA([128, 4, 512], BF16, "od_sb")
    mg_sb = [sbA([128, 8, 512], BF16, f"mg_sb{i}") for i in range(2)]
    t1_sb = [sbA([128, 512], F32, f"t1_sb{i}") for i in range(2)]
    t2_sb = [sbA([128, 512], F32, f"t2_sb{i}") for i in range(2)]
    t3_sb = [sbA([128, 512], F32, f"t3_sb{i}") for i in range(2)]
    xs_sb = [sbA([128, 1024], F32, f"xs_sb{i}") for i in range(2)]
    xTf_sb = sbA([128, 8, 128], F32, "xTf_sb")
    xTb_sb = [sbA([128, 8, 128], BF16, f"xTb_sb{i}") for i in range(2)]
    rt_sb = [sbA([128, 96], F32, f"rt_sb{i}") for i in range(2)]
    rs_sb = [sbA([128, 16], F32, f"rs_sb{i}") for i in range(2)]
    G_sb = [sbA([128, 32], F32, f"G_sb{i}") for i in range(2)]
    P.dma("pool", lambda e: e.dma_start(out=dsaout_sb[:], in_=dsaout.rearrange("(k p) n -> p k n", p=128)), w=["dsaout"])
    P.dma("pool", lambda e: e.dma_start(out=mlaout_sb[:], in_=mlaout.rearrange("(k p) n -> p k n", p=128)), w=["mlaout"])
    P.dma("pool", lambda e: e.dma_start(out=wo_sb[:], in_=wo.rearrange("(k p) n -> p k n", p=128)), w=["wo"])
    P.dma("sp", lambda e: e.dma_start(out=rw_sb[:], in_=rw.rearrange("(k p) n -> p k n", p=128)), w=["rw"])
    P.dma("sp", lambda e: e.dma_start(out=rb_sb[:], in_=rb.partition_broadcast(128)), w=["rb"])
    P.dma("sp", lambda e: e.dma_start(out=idn_sb[:], in_=idn), w=["idn"])
    chk(1)
    si = 0
    for tt in range(NT3 // 512):
        cs = slice(tt * 512, (tt + 1) * 512)
        P.dma("sp", lambda e: e.dma_start(out=ma_sb[:], in_=ma[:, cs].rearrange("(m p) n -> p m n", p=128)), w=["ma"])
        P.dma("act", lambda e: e.dma_start(out=mb_sb[:], in_=mb[:, cs].rearrange("(m p) n -> p m n", p=128)), w=["mb"])
        P.dma("sp", lambda e: e.dma_start(out=g2_sb[:], in_=g2[:, cs].rearrange("(m p) n -> p m n", p=128)), w=["g2"])
        P.dma("act", lambda e: e.dma_start(out=g3_sb[:], in_=g3[:, cs].rearrange("(m p) n -> p m n", p=128)), w=["g3"])
        P.dma("sp", lambda e: e.dma_start(out=oc_sb[:], in_=oc[:, cs].rearrange("(m p) n -> p m n", p=128)), w=["oc"])
        P.dma("act", lambda e: e.dma_start(out=od_sb[:], in_=od[:, cs].rearrange("(m p) n -> p m n", p=128)), w=["od"])
        mg, mgk = mg_sb[tt % 2], f"mg{tt % 2}"
        for m in range(8):
            pc, pck = newps()
            pd, pdk = newps()
            for k in range(4):
                P.pe(lambda e, k=k: e.matmul(pc[:, :], lhsT=dsaout_sb[:, k, m * 128:(m + 1) * 128], rhs=oc_sb[:, k, :], start=(k == 0), stop=(k == 3)),
                     r=["dsaout", "oc"], w=[pck])
            for k in range(4):
                P.pe(lambda e, k=k: e.matmul(pd[:, :], lhsT=mlaout_sb[:, k, m * 128:(m + 1) * 128], rhs=od_sb[:, k, :], start=(k == 0), stop=(k == 3)),
                     r=["mlaout", "od"], w=[pdk])
            t1, t1k = t1_sb[m % 2], f"t1{m % 2}"
            t2, t2k = t2_sb[m % 2], f"t2{m % 2}"
            t3, t3k = t3_sb[m % 2], f"t3{m % 2}"
            P.dve(lambda e: e.tensor_tensor(out=t1[:], in0=pc[:, :], in1=g2_sb[:, m, :], op=ALU.mult), r=[pck, "g2"], w=[t1k])
            P.dve(lambda e: e.tensor_tensor(out=t2[:], in0=pd[:, :], in1=g3_sb[:, m, :], op=ALU.mult), r=[pdk, "g3"], w=[t2k])
            P.pool(lambda e: e.tensor_tensor(out=t3[:], in0=ma_sb[:, m, :], in1=mb_sb[:, m, :], op=ALU.add), r=["ma", "mb"], w=[t3k])
            P.pool(lambda e: e.tensor_tensor(out=t1[:], in0=t1[:], in1=t2[:], op=ALU.add), r=[t1k, t2k], w=[t1k])
            P.pool(lambda e: e.tensor_tensor(out=mg[:, m, :], in0=t1[:], in1=t3[:], op=ALU.add), r=[t1k, t3k], w=[mgk])
        chk(2)
        for s in range(4):
            tok0 = tt * 512 + s * 128
            xs, xsk = xs_sb[si % 2], f"xs{si % 2}"
            xTb, xTbk = xTb_sb[si % 2], f"xTb{si % 2}"
            rt, rtk = rt_sb[si % 2], f"rt{si % 2}"
            rs, rsk = rs_sb[si % 2], f"rs{si % 2}"
            Gs, Gsk = G_sb[si % 2], f"G{si % 2}"
            si += 1
            P.dma("sp", lambda e: e.dma_start(out=xs[:], in_=x[tok0:tok0 + 128, :]), w=[xsk])
            u, uk = u_r.next()
            for half in range(2):
                pz, pzk = newps()
                for k in range(8):
                    P.pe(lambda e, k=k: e.matmul(pz[:, :], lhsT=mg[:, k, s * 128:(s + 1) * 128], rhs=wo_sb[:, k, half * 512:(half + 1) * 512],
                                                 start=(k == 0), stop=(k == 7)), r=[mgk, "wo"], w=[pzk])
                P.dve(lambda e: e.scalar_tensor_tensor(out=u[:, half * 512:(half + 1) * 512], in0=xs[:, half * 512:(half + 1) * 512], scalar=ALPHA,
                                                       in1=pz[:, :], op0=ALU.mult, op1=ALU.add), r=[xsk, pzk], w=[uk])
            x1s, x1k = x1s_r.next()
            layernorm(u[:], uk, gb_sb, "gb", 0, x1s[:], x1k, lntmp())
            chk(3)
            P.dma("sp", lambda e: e.dma_start(out=x1_d[tok0:tok0 + 128, :], in_=x1s[:]), r=[x1k], w=["x1_d"])
            chk(31)
            for hb in range(2):
                pt, ptk = newps()
                for q in range(4):
                    k = hb * 4 + q
                    P.pe(lambda e, k=k, q=q: e.matmul(pt[:, q * 128:(q + 1) * 128], lhsT=x1s[:, k * 128:(k + 1) * 128], rhs=idn_sb[:], start=True, stop=True),
                         r=[x1k, "idn"], w=[ptk])
                chk(32)
                for q in range(4):
                    k = hb * 4 + q
                    P.act(lambda e, k=k, q=q: e.copy(out=xTf_sb[:, k, :], in_=pt[:, q * 128:(q + 1) * 128]), r=[ptk], w=["xTf"])
            P.dve(lambda e: e.tensor_copy(out=xTb[:], in_=xTf_sb[:]), r=["xTf"], w=[xTbk])
            chk(33)
            for k in range(8):
                P.dma("act" if k % 2 else "sp", lambda e, k=k: e.dma_start(out=x1T_d[k * 128:(k + 1) * 128, tok0:tok0 + 128], in_=xTb[:, k, :]), r=[xTbk], w=["x1T_d"])
            chk(4)
            pr, prk = newps()
            for k in range(8):
                P.pe(lambda e, k=k: e.matmul(pr[:, 0:32], lhsT=xTf_sb[:, k, :], rhs=rw_sb[:, k, :], start=(k == 0), stop=(k == 7)),
                     r=["xTf", "rw"], w=[prk])
            P.dve(lambda e: e.tensor_tensor(out=rt[:, 0:32], in0=pr[:, 0:32], in1=rb_sb[:], op=ALU.add), r=[prk, "rb"], w=[rtk])
            P.dve(lambda e: e.max(out=rs[:, 0:8], in_=rt[:, 0:32]), r=[rtk], w=[rsk])
            P.dve(lambda e: e.tensor_scalar(out=rs[:, 8:9], in0=rs[:, 0:1], scalar1=-1.0, scalar2=None, op0=ALU.mult), r=[rsk], w=[rsk])
            P.act(lambda e: e.activation(out=rt[:, 32:64], in_=rt[:, 0:32], func=AF.Exp, bias=rs[:, 8:9], scale=1.0), r=[rtk, rsk], w=[rtk])
            P.dve(lambda e: e.scalar_tensor_tensor(out=rt[:, 64:96], in0=rt[:, 0:32], scalar=rs[:, 3:4], in1=rt[:, 32:64], op0=ALU.is_ge, op1=ALU.mult),
                  r=[rtk, rsk], w=[rtk])
            P.dve(lambda e: e.tensor_reduce(out=rs[:, 9:10], in_=rt[:, 64:96], axis=AX.X, op=ALU.add), r=[rtk], w=[rsk])
            P.dve(lambda e: e.reciprocal(out=rs[:, 10:11], in_=rs[:, 9:10]), r=[rsk], w=[rsk])
            P.dve(lambda e: e.tensor_scalar(out=Gs[:], in0=rt[:, 64:96], scalar1=rs[:, 10:11], scalar2=None, op0=ALU.mult), r=[rtk, rsk], w=[Gsk])
            P.dma("sp", lambda e: e.dma_start(out=G_d[tok0:tok0 + 128, :], in_=Gs[:]), r=[Gsk], w=["G_d"])
            chk(5)
    chk(6)
    P.barrier()
    stA.close()

    wgu_sb = [P.sb([128, 8, 2048], BF16, f"wgu_sb{i}") for i in range(2)]
    wd_sb = [P.sb([128, 8, 1024], BF16, f"wd_sb{i}") for i in range(1)]
    bgu_sb = [P.sb([128, 16], F32, f"bgu_sb{i}") for i in range(2)]
    bd_sb = [P.sb([1, 1024], BF16, f"bd_sb{i}") for i in range(2)]
    ones_bf = P.sb([1, 128], BF16, "ones_bf")
    xT_sb = P.sb([128, 8, 1024], BF16, "xT_sb")
    acc_sb = P.sb([128, 8, 1024], F32, "acc_sb")
    Ga_sb = P.sb([128, 8, 32], F32, "Ga_sb")
    actT = [P.sb([128, 8, 512], BF16, f"actT{i}") for i in range(2)]
    gc_r = Ring(P, 2, [128, 512], F32, "gc")
    sg_r = Ring(P, 2, [128, 512], F32, "sg")
    l1_r = Ring(P, 2, [128, 512], F32, "l1")
    P.dve(lambda e: e.memset(ones_bf[:], 1.0), w=["ones_bf"])
    ei = 0
    ai = 0
    for sp in range(NT3 // 1024):
        t0 = sp * 1024
        P.dma("sp", lambda e: e.dma_start(out=xT_sb[:], in_=x1T_d[:, t0:t0 + 1024].rearrange("(k p) n -> p k n", p=128)), r=["x1T_d"], w=["xT"])
        P.dma("act", lambda e: e.dma_start(out=Ga_sb[:], in_=G_d[t0:t0 + 1024, :].rearrange("(s p) g -> p s g", p=128)), r=["G_d"], w=["Ga"])
        P.pool(lambda e: e.memset(acc_sb[:], 0.0), w=["acc"])
        for ex in range(NEXP):
            wg, wgk = wgu_sb[ei % 2], f"wgu{ei % 2}"
            wdd, wdk = wd_sb[0], "wd0"
            bg, bgk = bgu_sb[ei % 2], f"bgu{ei % 2}"
            bdd, bdk = bd_sb[ei % 2], f"bd{ei % 2}"
            ei += 1
            for hf in range(2):
                P.dma("pool", lambda e, hf=hf: e.dma_start(out=wg[:, :, hf * 1024:(hf + 1) * 1024],
                                                           in_=wgu[ex * 1024:(ex + 1) * 1024, hf * 1024:(hf + 1) * 1024].rearrange("(k p) n -> p k n", p=128)),
                      w=[wgk])
            P.dma("pool", lambda e: e.dma_start(out=wdd[:], in_=wd[ex * 1024:(ex + 1) * 1024, :].rearrange("(k p) n -> p k n", p=128)), w=[wdk])
            P.dma("sp", lambda e: e.dma_start(out=bg[:], in_=bgu[ex * 128:(ex + 1) * 128, :]), w=[bgk])
            P.dma("pool", lambda e: e.dma_start(out=bdd[:], in_=bd[ex:ex + 1, :]), w=[bdk])
            chk(7)
            for t2 in range(2):
                aT, aTk = actT[ai % 2], f"actT{ai % 2}"
                ai += 1
                for mgi in range(8):
                    pg, pgk = newps()
                    pl, plk = newps()
                    for k in range(8):
                        P.pe(lambda e, k=k: e.matmul(pg[:, :], lhsT=wg[:, k, mgi * 128:(mgi + 1) * 128], rhs=xT_sb[:, k, t2 * 512:(t2 + 1) * 512],
                                                     start=(k == 0), stop=(k == 7)), r=[wgk, "xT"], w=[pgk])
                    for k in range(8):
                        P.pe(lambda e, k=k: e.matmul(pl[:, :], lhsT=wg[:, k, 1024 + mgi * 128:1024 + (mgi + 1) * 128], rhs=xT_sb[:, k, t2 * 512:(t2 + 1) * 512],
                                                     start=(k == 0), stop=(k == 7)), r=[wgk, "xT"], w=[plk])
                    gc, gck = gc_r.next()
                    sg, sgk = sg_r.next()
                    l1, l1k = l1_r.next()
                    P.dve(lambda e: e.tensor_scalar(out=gc[:], in0=pg[:, :], scalar1=bg[:, mgi:mgi + 1], scalar2=LIM, op0=ALU.add, op1=ALU.min),
                          r=[pgk, bgk], w=[gck])
                    P.act(lambda e: e.activation(out=sg[:], in_=gc[:], func=AF.Sigmoid, scale=SWA), r=[gck], w=[sgk])
                    P.dve(lambda e: e.tensor_scalar(out=l1[:], in0=pl[:, :], scalar1=bg[:, 8 + mgi:9 + mgi], scalar2=LIM, op0=ALU.add, op1=ALU.min),
                          r=[plk, bgk], w=[l1k])
                    P.pool(lambda e: e.tensor_scalar(out=l1[:], in0=l1[:], scalar1=-LIM, scalar2=1.0, op0=ALU.max, op1=ALU.add), r=[l1k], w=[l1k])
                    P.pool(lambda e: e.tensor_tensor(out=sg[:], in0=gc[:], in1=sg[:], op=ALU.mult), r=[gck, sgk], w=[sgk])
                    P.dve(lambda e: e.tensor_tensor(out=aT[:, mgi, :], in0=sg[:], in1=l1[:], op=ALU.mult), r=[sgk, l1k], w=[aTk])
                for s in range(4):
                    sub = t2 * 4 + s
                    for half in range(2):
                        py, pyk = newps()
                        for k in range(8):
                            P.pe(lambda e, k=k: e.matmul(py[:, :], lhsT=aT[:, k, s * 128:(s + 1) * 128], rhs=wdd[:, k, half * 512:(half + 1) * 512],
                                                         start=(k == 0), stop=False), r=[aTk, wdk], w=[pyk])
                        P.pe(lambda e: e.matmul(py[:, :], lhsT=ones_bf[0:1, :], rhs=bdd[0:1, half * 512:(half + 1) * 512], start=False, stop=True),
                             r=["ones_bf", bdk], w=[pyk])
                        P.dve(lambda e: e.scalar_tensor_tensor(out=acc_sb[:, sub, half * 512:(half + 1) * 512], in0=py[:, :], scalar=Ga_sb[:, sub, ex:ex + 1],
                                                               in1=acc_sb[:, sub, half * 512:(half + 1) * 512], op0=ALU.mult, op1=ALU.add),
                              r=[pyk, "Ga", "acc"], w=["acc"])
        chk(8)
        for sub in range(8):
            tok0 = t0 + sub * 128
            x1s, x1k = x1s_r.next()
            P.dma("sp", lambda e: e.dma_start(out=x1s[:], in_=x1_d[tok0:tok0 + 128, :]), r=["x1_d"], w=[x1k])
            u, uk = u_r.next()
            P.dve(lambda e: e.scalar_tensor_tensor(out=u[:], in0=x1s[:], scalar=ALPHA, in1=acc_sb[:, sub, :], op0=ALU.mult, op1=ALU.add),
                  r=[x1k, "acc"], w=[uk])
            x2s, x2k = x1s_r.next()
            layernorm(u[:], uk, gb_sb, "gb", 2, x2s[:], x2k, lntmp())
            P.dma("sp", lambda e: e.dma_start(out=o_x2[tok0:tok0 + 128, :], in_=x2s[:]), r=[x2k])
    nc = P.finish()
    return nc, P.stats


def fm16(v):
    return np.ascontiguousarray(v.reshape(16, 128).T)


_PROGS = {}


def _prog(name, fn):
    if name not in _PROGS:
        _PROGS[name] = fn()[0]
    return _PROGS[name]


def kernel(**inputs):
    inp = {k: np.asarray(v) for k, v in inputs.items()}
    x_cur = np.ascontiguousarray(inp["x"], dtype=np.float32)
    idn_bf = np.eye(128, dtype=np.float32).astype(NPBF)
    idn_f = np.eye(128, dtype=np.float32)
    QC = [qcols(0), qcols(1)]
    for l in range(2):
        inp["x_cur"] = x_cur
        r1 = run_bass_kernel_spmd(_prog("p1", build_p1), prep_p1(inp, l, None), core_ids=list(range(8))).results
        r1 = [{k: np.asarray(v) for k, v in r.items()} for r in r1]

        def cat(b, k, axis):
            return np.concatenate([r1[2 * b][k], r1[2 * b + 1][k]], axis=axis)

        m_maps, d_maps = [], []
        for c in range(8):
            b, half = c // 2, c % 2
            qc = QC[half]
            m_maps.append(dict(qn=np.ascontiguousarray(cat(b, "qn", 1)[:, qc]), qr=relayout_qr(cat(b, "qr", 1)[:, qc]),
                               kn=cat(b, "kn", 1), kr=cat(b, "kr", 1), vm=cat(b, "vm", 0), msk=make_masks(half), idn=idn_bf))
            d_maps.append(dict(qd=np.ascontiguousarray(cat(b, "qd", 1)[:, qc]), qi=relayout_qr(cat(b, "qi", 1)[:, qc]),
                               kd=cat(b, "kd", 1), vd=cat(b, "vd", 0), ki=cat(b, "ki", 1),
                               iw=np.ascontiguousarray(cat(b, "iw", 0)[qc]), cmsk=make_cmask(half), idn=idn_bf))
        rm = run_bass_kernel_spmd(_prog("mla", build_mla), m_maps, core_ids=list(range(8))).results
        rd = run_bass_kernel_spmd(_prog("dsa", build_dsa), d_maps, core_ids=list(range(8))).results
        od_full, oc_full = [], []
        for b in range(4):
            om = np.zeros((512, S), NPBF)
            oc_ = np.zeros((512, S), NPBF)
            for half in range(2):
                om[:, QC[half]] = np.asarray(rm[2 * b + half]["o"])
                oc_[:, QC[half]] = np.asarray(rd[2 * b + half]["o"])
            od_full.append(om)
            oc_full.append(oc_)
        lnv = np.concatenate([inp["ln1_g"][l], inp["ln1_b"][l], inp["ln2_g"][l], inp["ln2_b"][l]]).reshape(1, 4096).astype(np.float32)
        bgu = np.ascontiguousarray(np.stack([fm16(inp["exp_b_gu"][l][e]) for e in range(32)]).reshape(32 * 128, 16))
        wgu = np.ascontiguousarray(inp["exp_w_gu"][l].reshape(32 * 1024, 2048))
        wd = np.ascontiguousarray(inp["exp_w_d"][l].reshape(32 * 1024, 1024))
        maps3 = []
        for c in range(2):
            bs = (2 * c, 2 * c + 1)
            maps3.append(dict(
                ma=np.concatenate([cat(b, "ma", 1) for b in bs], axis=1), mb=np.concatenate([cat(b, "mb", 1) for b in bs], axis=1),
                g2=np.concatenate([cat(b, "g2", 1) for b in bs], axis=1), g3=np.concatenate([cat(b, "g3", 1) for b in bs], axis=1),
                oc=np.concatenate([oc_full[b] for b in bs], axis=1), od=np.concatenate([od_full[b] for b in bs], axis=1),
                x=np.ascontiguousarray(x_cur[2 * c:2 * c + 2].reshape(NT3, 1024)),
                dsaout=inp["dsa_out"][l], mlaout=inp["mla_out"][l], wo=inp["w_o"][l], lnv=lnv,
                rw=inp["router_w"][l], rb=inp["router_b"][l].reshape(1, 32), wgu=wgu, bgu=bgu, wd=wd, bd=inp["exp_b_d"][l], idn=idn_f))
        del r1, rm, rd
        r3 = run_bass_kernel_spmd(_prog("p3", build_p3), maps3, core_ids=[0, 1]).results
        x_cur = np.concatenate([np.asarray(r3[0]["x2"]), np.asarray(r3[1]["x2"])], axis=0).reshape(4, S, 1024).astype(np.float32)
        del maps3, r3
    return x_cur
```

```python
import math
import numpy as np
import ml_dtypes
from contextlib import ExitStack
import concourse.bass as bass
import concourse.mybir as mybir
from concourse.bass_utils import run_bass_kernel_spmd

F32 = mybir.dt.float32
BF16 = mybir.dt.bfloat16
I32 = mybir.dt.int32
AF = mybir.ActivationFunctionType
ALU = mybir.AluOpType
AX = mybir.AxisListType
NPBF = ml_dtypes.bfloat16

ENGS = ("pe", "act", "dve", "pool", "sp")
N_DMA_SEMS = 6


class Prog:
    def __init__(self, name="k"):
        self.nc = bass.Bass("TRN2", target_bir_lowering=False)
        self.es = ExitStack()
        self.ops = []
        self.n_sb = 0
        self.out_keys = []

    def dram_in(self, name, shape, dt):
        return self.nc.dram_tensor(name, list(shape), dt, kind="ExternalInput").ap()

    def dram_out(self, name, shape, dt):
        return self.nc.dram_tensor(name, list(shape), dt, kind="ExternalOutput").ap()

    def dram_tmp(self, name, shape, dt):
        return self.nc.dram_tensor(name, list(shape), dt, kind="Internal").ap()

    def sb(self, shape, dt, name=None):
        self.n_sb += 1
        return self.es.enter_context(self.nc.sbuf_tensor(name or f"sb{self.n_sb}", list(shape), dt))

    def ps(self, shape, dt=F32, name=None):
        self.n_sb += 1
        return self.es.enter_context(self.nc.psum_tensor(name or f"ps{self.n_sb}", list(shape), dt))

    def _init_emit(self):
        nc = self.nc
        self.engobj = {"pe": nc.tensor, "act": nc.scalar, "dve": nc.vector, "pool": nc.gpsimd, "sp": nc.sync}
        self.sems = {e: self.es.enter_context(nc.semaphore(f"s_{e}")) for e in ENGS}
        self.dsems = {}
        self.cnt = {e: 0 for e in ENGS}
        self.dcnt = {}
        self.dma_n = {e: 0 for e in ENGS}
        self.slot_last = {}
        self.lastw = {}
        self.readers = {}
        self.known = {e: {} for e in ENGS}
        self.n_wait = 0
        self.n_ops = 0

    def op(self, eng, fn, reads=(), writes=(), dma=False):
        if not hasattr(self, "engobj"):
            self._init_emit()
        if getattr(self, "dead", False):
            return
        E = self.engobj[eng]
        need = []
        for k in reads:
            sg = self.lastw.get(k)
            if sg is not None:
                need.append(sg)
        for k in writes:
            sg = self.lastw.get(k)
            if sg is not None:
                need.append(sg)
            need.extend(self.readers.get(k, ()))
        slot = None
        if dma:
            slot = (eng, self.dma_n[eng] % N_DMA_SEMS)
            self.dma_n[eng] += 1
            if slot not in self.dsems:
                self.dsems[slot] = self.es.enter_context(self.nc.semaphore(f"d_{eng}{slot[1]}"))
            if slot in self.slot_last:
                need.append(self.slot_last[slot])
        best = {}
        for sg in need:
            name, h, v, src, sdma = sg
            if (not dma) and (not sdma) and src == "pe" and eng == "pe":
                continue
            if self.known[eng].get(name, 0) >= v:
                continue
            if name not in best or best[name][2] < v:
                best[name] = sg
        for name, sg in best.items():
            E.wait_ge(sg[1], sg[2])
            self.known[eng][name] = sg[2]
            self.n_wait += 1
        inst = fn(E)
        self.n_ops += 1
        if dma:
            self.dcnt[slot] = self.dcnt.get(slot, 0) + 16
            inst.then_inc(self.dsems[slot], 16)
            mysig = (f"d_{slot}", self.dsems[slot], self.dcnt[slot], eng, True)
            self.slot_last[slot] = mysig
        else:
            self.cnt[eng] += 1
            inst.then_inc(self.sems[eng], 1)
            mysig = (f"s_{eng}", self.sems[eng], self.cnt[eng], eng, False)
        for k in writes:
            self.lastw[k] = mysig
            self.readers[k] = []
        for k in reads:
            if k not in writes:
                self.readers.setdefault(k, []).append(mysig)

    def pe(self, fn, r=(), w=()):
        self.op("pe", fn, r, w)

    def act(self, fn, r=(), w=()):
        self.op("act", fn, r, w)

    def dve(self, fn, r=(), w=()):
        self.op("dve", fn, r, w)

    def pool(self, fn, r=(), w=()):
        self.op("pool", fn, r, w)

    def dma(self, q, fn, r=(), w=()):
        self.op(q, fn, r, w, dma=True)

    def barrier(self):
        if not hasattr(self, "engobj"):
            self._init_emit()
        for e in ENGS:
            E = self.engobj[e]
            for f in ("pe", "act", "dve", "pool"):
                if self.cnt[f] and self.known[e].get(f"s_{f}", 0) < self.cnt[f]:
                    E.wait_ge(self.sems[f], self.cnt[f])
                    self.known[e][f"s_{f}"] = self.cnt[f]
            for slot, v in self.dcnt.items():
                nm = f"d_{slot}"
                if self.known[e].get(nm, 0) < v:
                    E.wait_ge(self.dsems[slot], v)
                    self.known[e][nm] = v

    def finish(self):
        nc = self.nc
        for slot, v in self.dcnt.items():
            nc.sync.wait_ge(self.dsems[slot], v)
        for e in ("pe", "act", "dve", "pool"):
            if self.cnt[e]:
                nc.sync.wait_ge(self.sems[e], self.cnt[e])
        self.stats = dict(n_ops=self.n_ops, n_wait=self.n_wait, cnt=dict(self.cnt))
        self.es.close()
        return nc


T, H, TT, NT = 4096, 32, 512, 8
EPS = 1e-5
THETA = 500000.0
O_POOL, O_CA, O_CG, O_CQ, O_CK, O_CV, O_IQ, O_IK, O_IW, O_MQ, O_MKV, O_MKR, O_G = (
    0, 512, 1024, 1536, 2048, 2560, 3072, 3328, 3360, 3368, 3752, 4008, 4040)
PI = math.pi


class Ring:
    def __init__(self, P, n, shape, dt, name):
        self.t = [P.sb(shape, dt, name=f"{name}{i}") for i in range(n)]
        self.k = [f"{name}{i}" for i in range(n)]
        self.i = 0

    def next(self):
        j = self.i % len(self.t)
        self.i += 1
        return self.t[j], self.k[j]


U_POOL, U_CA, U_CG, U_CQ, U_CQR, U_CK, U_CKR, U_IQ, U_IQR, U_IK, U_IKR, U_KR, U_KRR, U_MQ, U_MKV = (
    0, 4, 8, 12, 16, 20, 24, 28, 30, 32, 33, 34, 35, 36, 39)
U_G0, U_G1, U_G2, U_G3, U_CV, U_IW, NU = 41, 49, 57, 65, 73, 77, 78


def rotperm(nh, hd, rot):
    half = rot // 2
    idx = np.arange(nh * hd)
    for h in range(nh):
        for j in range(half):
            idx[h * hd + j] = h * hd + j + half
            idx[h * hd + j + half] = h * hd + j
    return idx


def rope_consts():
    c = np.zeros((128, 8), np.float32)
    for ty, (hd, rot) in enumerate([(64, 16), (32, 8), (32, 32)]):
        half = rot // 2
        inv = (np.float32(THETA) ** (-np.arange(0, rot, 2, dtype=np.float32) / np.float32(rot))).astype(np.float32)
        for p in range(128):
            d = p % hd
            if d < rot:
                c[p, ty] = inv[d % half]
                c[p, 3 + ty] = -1.0 if d < half else 1.0
    return c


def build_p1():
    P = Prog()
    xT = P.dram_in("xT", [1024, H + T], F32)
    W = P.dram_in("W", [1024, NU * 128], F32)
    posd = P.dram_in("pos", [1, T], I32)
    cst = P.dram_in("cst", [128, 8], F32)
    pcorr = P.dram_in("pcorr", [128, 4 * 16], F32)
    poolw = P.dram_in("poolw", [128, 4 * 128], F32)
    pvec = P.dram_in("pvec", [128, 32], F32)
    poolout = P.dram_in("poolout", [512, 1024], F32)
    convw = P.dram_in("convw", [128, 4 * 31], F32)
    convout = P.dram_in("convout", [512, 1024], F32)
    wuqn = P.dram_in("wuqn", [384, 512], F32)
    wuqr = P.dram_in("wuqr", [384, 512], F32)
    wuk = P.dram_in("wuk", [256, 512], F32)
    wuv = P.dram_in("wuv", [256, 512], F32)

    o_ma = P.dram_out("ma", [1024, T], F32)
    o_mb = P.dram_out("mb", [1024, T], F32)
    o_g2 = P.dram_out("g2", [1024, T], BF16)
    o_g3 = P.dram_out("g3", [1024, T], BF16)
    o_qd = P.dram_out("qd", [512, T], BF16)
    o_kd = P.dram_out("kd", [512, T], BF16)
    o_vd = P.dram_out("vd", [T, 520], BF16)
    o_qi = P.dram_out("qi", [256, T], BF16)
    o_ki = P.dram_out("ki", [128, T], BF16)
    o_iw = P.dram_out("iw", [T, 8], F32)
    o_qn = P.dram_out("qn", [512, T], BF16)
    o_qr = P.dram_out("qr", [256, T], BF16)
    o_kn = P.dram_out("kn", [512, T], BF16)
    o_kr = P.dram_out("kr", [128, T], BF16)
    o_vm = P.dram_out("vm", [T, 520], BF16)

    xb = P.sb([128, 8, H + T], BF16, "xb")
    posi_r = Ring(P, 2, [128, TT], I32, "posi")
    posf_r = Ring(P, 2, [128, TT], F32, "posf")
    cst_sb = P.sb([128, 8], F32, "cst_sb")
    pcorr_sb = P.sb([128, 64], F32, "pcorr_sb")
    pvec_sb = P.sb([128, 32], F32, "pvec_sb")
    poolw_sb = P.sb([128, 4, 128], BF16, "poolw_sb")
    poolout_sb = P.sb([128, 4, 1024], BF16, "poolout_sb")
    convw_sb = P.sb([128, 4, 31], F32, "convw_sb")
    convout_sb = P.sb([128, 4, 1024], BF16, "convout_sb")
    wuqn_sb = P.sb([128, 3, 512], BF16, "wuqn_sb")
    wuqr_sb = P.sb([128, 3, 512], BF16, "wuqr_sb")
    wuk_sb = P.sb([128, 2, 512], BF16, "wuk_sb")
    wuv_sb = P.sb([128, 2, 512], BF16, "wuv_sb")
    ones_sb = P.sb([128, 128], F32, "ones_sb")
    wring = [P.sb([128, 8, 512], BF16, f"wr{i}") for i in range(4)]

    for k in range(8):
        P.dma("pool", lambda e, k=k: e.dma_start(out=xb[:, k, :], in_=xT[k * 128:(k + 1) * 128, :]), w=["xb"])
    P.dma("sp", lambda e: e.dma_start(out=cst_sb[:], in_=cst), w=["cst"])
    P.dma("sp", lambda e: e.dma_start(out=pcorr_sb[:], in_=pcorr), w=["pcorr"])
    P.dma("sp", lambda e: e.dma_start(out=pvec_sb[:], in_=pvec), w=["pvec"])
    P.dma("sp", lambda e: e.dma_start(out=convw_sb[:], in_=convw.rearrange("p (c j) -> p c j", j=31)), w=["convw"])
    P.dma("pool", lambda e: e.dma_start(out=poolw_sb[:], in_=poolw.rearrange("p (g d) -> p g d", d=128)), w=["poolw"])
    P.dma("pool", lambda e: e.dma_start(out=poolout_sb[:], in_=poolout.rearrange("(k p) n -> p k n", p=128)), w=["poolout"])
    P.dma("pool", lambda e: e.dma_start(out=convout_sb[:], in_=convout.rearrange("(k p) n -> p k n", p=128)), w=["convout"])
    P.dma("pool", lambda e: e.dma_start(out=wuqn_sb[:], in_=wuqn.rearrange("(k p) n -> p k n", p=128)), w=["wuqn"])
    P.dma("pool", lambda e: e.dma_start(out=wuqr_sb[:], in_=wuqr.rearrange("(k p) n -> p k n", p=128)), w=["wuqr"])
    P.dma("pool", lambda e: e.dma_start(out=wuk_sb[:], in_=wuk.rearrange("(k p) n -> p k n", p=128)), w=["wuk"])
    P.dma("pool", lambda e: e.dma_start(out=wuv_sb[:], in_=wuv.rearrange("(k p) n -> p k n", p=128)), w=["wuv"])
    P.dve(lambda e: e.memset(ones_sb[:], 1.0), w=["ones"])

    PS = [P.ps([128, 512], F32, name=f"psb{i}") for i in range(8)]
    pctr = [0]

    def newps():
        i = pctr[0] % 8
        pctr[0] += 1
        return PS[i], f"ps{i}"

    wctr = [0]

    def loadw(u0):
        i = wctr[0] % 4
        wctr[0] += 1
        wt, wk = wring[i], f"wr{i}"
        nu = min(4, NU - u0)
        P.dma("pool", lambda e: e.dma_start(out=wt[:, :, 0:nu * 128],
                                            in_=W[:, u0 * 128:(u0 + nu) * 128].rearrange("(k p) n -> p k n", p=128)), w=[wk])
        return wt, wk

    def proj(wt, wk, ul, c0, ntok, ncol=128):
        ps, pk = newps()
        for k in range(8):
            P.pe(lambda e, k=k: e.matmul(ps[0:ncol, 0:ntok], lhsT=wt[:, k, ul * 128:ul * 128 + ncol],
                                         rhs=xb[:, k, c0:c0 + ntok], start=(k == 0), stop=(k == 7)),
                 r=[wk, "xb"], w=[pk])
        return ps, pk

    tmpA = Ring(P, 2, [128, 544], F32, "tA")
    tmpB = Ring(P, 2, [128, 544], F32, "tB")
    tmpC = Ring(P, 2, [128, 544], F32, "tC")
    ob32 = Ring(P, 2, [128, 512], F32, "ob32")
    ob16 = Ring(P, 2, [128, 512], BF16, "ob16")
    tabC = [P.sb([128, 512], F32, f"tabC{i}") for i in range(3)]
    tabS = [P.sb([128, 512], F32, f"tabS{i}") for i in range(3)]
    kint = P.sb([128, 512], I32, "kint")
    kflt = P.sb([128, 512], F32, "kflt")

    def make_tables(tt, types):
        posi, pik = posi_r.next()
        posf, pfk = posf_r.next()
        P.dma("sp", lambda e: e.dma_start(out=posi[:], in_=posd[:, tt * TT:(tt + 1) * TT].partition_broadcast(128)), w=[pik])
        P.dve(lambda e: e.tensor_copy(out=posf[:], in_=posi[:]), r=[pik], w=[pfk])
        for ty in types:
            for (tab, nm, ph) in ((tabS[ty], f"tabS{ty}", 0.0), (tabC[ty], f"tabC{ty}", 0.5 * PI)):
                P.dve(lambda e, tab=tab, ty=ty, ph=ph: e.tensor_scalar(
                    out=tab[:], in0=posf[:], scalar1=cst_sb[:, ty:ty + 1], scalar2=ph,
                    op0=ALU.mult, op1=ALU.add), r=[pfk, "cst"], w=[nm])
                P.dve(lambda e, tab=tab: e.tensor_scalar(out=kint[:], in0=tab[:], scalar1=1.0 / (2 * PI), scalar2=None,
                                                         op0=ALU.mult), r=[nm], w=["kint"])
                P.dve(lambda e: e.tensor_copy(out=kflt[:], in_=kint[:]), r=["kint"], w=["kflt"])
                P.dve(lambda e, tab=tab: e.scalar_tensor_tensor(out=tab[:], in0=kflt[:], scalar=-6.28125, in1=tab[:],
                                                                op0=ALU.mult, op1=ALU.add), r=[nm, "kflt"], w=[nm])
                P.dve(lambda e, tab=tab: e.scalar_tensor_tensor(out=tab[:], in0=kflt[:], scalar=-(2 * PI - 6.28125), in1=tab[:],
                                                                op0=ALU.mult, op1=ALU.add), r=[nm, "kflt"], w=[nm])
                P.dve(lambda e, tab=tab: e.tensor_scalar(out=tab[:], in0=tab[:], scalar1=-3.141592, scalar2=3.141592,
                                                         op0=ALU.max, op1=ALU.min), r=[nm], w=[nm])
                P.act(lambda e, tab=tab: e.activation(out=tab[:], in_=tab[:], func=AF.Sin), r=[nm], w=[nm])

    def rope_out(ps1, pk1, ps2, pk2, ty, dst, nrow=128):
        t1, k1 = tmpA.next()
        t2, k2 = tmpB.next()
        ob, ok = ob16.next()
        P.dve(lambda e: e.tensor_tensor(out=t1[:, 0:TT], in0=ps1[:, 0:TT], in1=tabC[ty][:], op=ALU.mult),
              r=[pk1, f"tabC{ty}"], w=[k1])
        P.dve(lambda e: e.scalar_tensor_tensor(out=t2[:, 0:TT], in0=ps2[:, 0:TT], scalar=cst_sb[:, 3 + ty:4 + ty],
                                               in1=tabS[ty][:], op0=ALU.mult, op1=ALU.mult),
              r=[pk2, f"tabS{ty}", "cst"], w=[k2])
        P.pool(lambda e: e.tensor_tensor(out=ob[:], in0=t1[:, 0:TT], in1=t2[:, 0:TT], op=ALU.add), r=[k1, k2], w=[ok])
        P.dma("sp", lambda e: e.dma_start(out=dst, in_=ob[0:nrow, :]), r=[ok])

    wtp, wkp = loadw(U_POOL)
    wg0a, wg0ak = loadw(U_G0)
    wg0b, wg0bk = loadw(U_G0 + 4)
    mixed = [P.sb([128, 4, 512], BF16, f"mixed{i}") for i in range(1)]
    pooled_r = Ring(P, 2, [128, 512], BF16, "pooled")
    WINS = (2, 4, 8, 16)
    for tt in range(NT):
        c0 = H + tt * TT
        mx, mxk = mixed[0], "mixed0"
        for g in range(4):
            psm, pkm = proj(wtp, wkp, g, c0, TT)
            psh, pkh = proj(wtp, wkp, g, c0 - 16, 16)
            U, uk = tmpA.next()
            A, ak = tmpB.next()
            B, bk = tmpC.next()
            P.act(lambda e: e.copy(out=U[:, 16:528], in_=psm[:, 0:512]), r=[pkm], w=[uk])
            P.act(lambda e: e.copy(out=U[:, 0:16], in_=psh[:, 0:16]), r=[pkh], w=[uk])
            src, sk = U, uk
            dsts = [(A, ak), (B, bk)]
            sh = 1
            lo = 1
            for wi in range(g + 1):
                dst, dk = dsts[wi % 2]
                P.dve(lambda e, dst=dst, src=src, lo=lo, sh=sh: e.tensor_tensor(
                    out=dst[:, lo:528], in0=src[:, lo:528], in1=src[:, lo - sh:528 - sh], op=ALU.add), r=[sk], w=[dk])
                src, sk = dst, dk
                sh *= 2
                lo += sh
            other, okk = dsts[(g + 1) % 2]
            P.dve(lambda e: e.tensor_scalar(out=other[:, 16:528], in0=src[:, 16:528], scalar1=1.0 / WINS[g], scalar2=None,
                                            op0=ALU.mult), r=[sk], w=[okk])
            if tt == 0:
                P.dve(lambda e: e.tensor_tensor(out=other[:, 16:32], in0=other[:, 16:32], in1=pcorr_sb[:, g * 16:(g + 1) * 16],
                                                op=ALU.mult), r=[okk, "pcorr"], w=[okk])
            pl, plk = pooled_r.next()
            P.dve(lambda e: e.tensor_tensor(out=pl[:], in0=other[:, 16:528], in1=U[:, 16:528], op=ALU.subtract),
                  r=[okk, uk], w=[plk])
            ps2, pk2 = newps()
            P.pe(lambda e: e.matmul(ps2[:, :], lhsT=poolw_sb[:, g, :], rhs=pl[:], start=True, stop=True),
                 r=["poolw", plk], w=[pk2])
            P.act(lambda e: e.activation(out=mx[:, g, :], in_=ps2[:, :], func=AF.Copy, scale=pvec_sb[:, g:g + 1]),
                  r=[pk2, "pvec"], w=[mxk])
        for m in range(8):
            psy, pky = newps()
            for g in range(4):
                P.pe(lambda e, g=g: e.matmul(psy[:, :], lhsT=poolout_sb[:, g, m * 128:(m + 1) * 128], rhs=mx[:, g, :],
                                             start=(g == 0), stop=(g == 3)), r=["poolout", mxk], w=[pky])
            wt, wk = (wg0a, wg0ak) if m < 4 else (wg0b, wg0bk)
            psg, pkg = proj(wt, wk, m % 4, c0, TT)
            sg, sgk = tmpA.next()
            ob, ok = ob32.next()
            P.act(lambda e: e.activation(out=sg[:, 0:TT], in_=psg[:, :], func=AF.Sigmoid), r=[pkg], w=[sgk])
            P.dve(lambda e: e.tensor_tensor(out=ob[:], in0=psy[:, :], in1=sg[:, 0:TT], op=ALU.mult), r=[pky, sgk], w=[ok])
            P.dma("sp", lambda e, m=m: e.dma_start(out=o_ma[m * 128:(m + 1) * 128, tt * TT:(tt + 1) * TT], in_=ob[:]), r=[ok])

    wca, wcak = loadw(U_CA)
    wcg, wcgk = loadw(U_CG)
    wg1a, wg1ak = loadw(U_G1)
    wg1b, wg1bk = loadw(U_G1 + 4)
    hbuf = Ring(P, 2, [128, 544], F32, "hbuf")
    accs = Ring(P, 2, [128, 512], F32, "acc")
    hc = [P.sb([128, 4, 512], F32, f"hc{i}") for i in range(1)]
    sq = [P.sb([128, 4, 512], F32, f"sq{i}") for i in range(1)]
    hs = [P.sb([128, 4, 512], BF16, f"hs{i}") for i in range(1)]
    P.dve(lambda e: e.memset(ones_sb[:], 1.0), w=["ones"])
    for tt in range(NT):
        c0 = H + tt * TT
        hcc, hck = hc[0], "hc0"
        sqq, sqk = sq[0], "sq0"
        hss, hsk = hs[0], "hs0"
        for c in range(4):
            pa, pak = proj(wca, wcak, c, c0, TT)
            pah, pahk = proj(wca, wcak, c, c0 - 32, 32)
            pg, pgk = proj(wcg, wcgk, c, c0, TT)
            pgh, pghk = proj(wcg, wcgk, c, c0 - 32, 32)
            sg, sgk = tmpA.next()
            hb, hbk = hbuf.next()
            P.act(lambda e: e.activation(out=sg[:, 32:544], in_=pg[:, 0:512], func=AF.Sigmoid), r=[pgk], w=[sgk])
            P.act(lambda e: e.activation(out=sg[:, 0:32], in_=pgh[:, 0:32], func=AF.Sigmoid), r=[pghk], w=[sgk])
            P.dve(lambda e: e.tensor_tensor(out=hb[:, 32:544], in0=pa[:, 0:512], in1=sg[:, 32:544], op=ALU.mult),
                  r=[pak, sgk], w=[hbk])
            P.dve(lambda e: e.tensor_tensor(out=hb[:, 0:32], in0=pah[:, 0:32], in1=sg[:, 0:32], op=ALU.mult),
                  r=[pahk, sgk], w=[hbk])
            a0, a0k = accs.next()
            P.dve(lambda e, c=c: e.tensor_scalar(out=a0[:], in0=hb[:, 2:514], scalar1=convw_sb[:, c, 0:1],
                                                 scalar2=pvec_sb[:, 4 + c:5 + c], op0=ALU.mult, op1=ALU.add),
                  r=[hbk, "convw", "pvec"], w=[a0k])
            cur, curk = a0, a0k
            for j in range(1, 31):
                if j == 30:
                    nxt, nxtk = hcc[:, c, :], hck
                else:
                    nx, nxtk = accs.next()
                    nxt = nx[:]
                P.dve(lambda e, c=c, j=j, cur=cur, nxt=nxt: e.scalar_tensor_tensor(
                    out=nxt, in0=hb[:, 2 + j:514 + j], scalar=convw_sb[:, c, j:j + 1], in1=cur[:] if j == 1 else cur,
                    op0=ALU.mult, op1=ALU.add), r=[hbk, "convw", curk], w=[nxtk])
                cur, curk = nxt, nxtk
            P.act(lambda e, c=c: e.activation(out=sqq[:, c, :], in_=hcc[:, c, :], func=AF.Square), r=[hck], w=[sqk])
        pmean, pmeank = newps()
        pmsq, pmsqk = newps()
        for c in range(4):
            P.pe(lambda e, c=c: e.matmul(pmean[:, :], lhsT=ones_sb[:], rhs=hcc[:, c, :], start=(c == 0), stop=(c == 3)),
                 r=["ones", hck], w=[pmeank])
        for c in range(4):
            P.pe(lambda e, c=c: e.matmul(pmsq[:, :], lhsT=ones_sb[:], rhs=sqq[:, c, :], start=(c == 0), stop=(c == 3)),
                 r=["ones", sqk], w=[pmsqk])
        mean, meank = tmpB.next()
        rstd, rstdk = tmpC.next()
        P.act(lambda e: e.activation(out=mean[:, 0:TT], in_=pmean[:, :], func=AF.Copy, scale=1.0 / 512), r=[pmeank], w=[meank])
        P.dve(lambda e: e.tensor_tensor(out=rstd[:, 0:TT], in0=mean[:, 0:TT], in1=mean[:, 0:TT], op=ALU.mult), r=[meank], w=[rstdk])
        P.dve(lambda e: e.scalar_tensor_tensor(out=rstd[:, 0:TT], in0=pmsq[:, :], scalar=1.0 / 512, in1=rstd[:, 0:TT],
                                               op0=ALU.mult, op1=ALU.subtract), r=[pmsqk, rstdk], w=[rstdk])
        P.dve(lambda e: e.tensor_scalar(out=rstd[:, 0:TT], in0=rstd[:, 0:TT], scalar1=EPS, scalar2=None, op0=ALU.add),
              r=[rstdk], w=[rstdk])
        P.act(lambda e: e.sqrt(out=rstd[:, 0:TT], in_=rstd[:, 0:TT]), r=[rstdk], w=[rstdk])
        P.dve(lambda e: e.reciprocal(out=rstd[:, 0:TT], in_=rstd[:, 0:TT]), r=[rstdk], w=[rstdk])
        for c in range(4):
            d1, d1k = tmpA.next()
            P.dve(lambda e, c=c: e.tensor_tensor(out=d1[:, 0:TT], in0=hcc[:, c, :], in1=mean[:, 0:TT], op=ALU.subtract),
                  r=[hck, meank], w=[d1k])
            P.dve(lambda e: e.tensor_tensor(out=d1[:, 0:TT], in0=d1[:, 0:TT], in1=rstd[:, 0:TT], op=ALU.mult), r=[d1k, rstdk], w=[d1k])
            P.act(lambda e, c=c: e.activation(out=hss[:, c, :], in_=d1[:, 0:TT], func=AF.Silu, scale=pvec_sb[:, 8 + c:9 + c],
                                              bias=pvec_sb[:, 12 + c:13 + c]), r=[d1k, "pvec"], w=[hsk])
        for m in range(8):
            psy, pky = newps()
            for c in range(4):
                P.pe(lambda e, c=c: e.matmul(psy[:, :], lhsT=convout_sb[:, c, m * 128:(m + 1) * 128], rhs=hss[:, c, :],
                                             start=(c == 0), stop=(c == 3)), r=["convout", hsk], w=[pky])
            wt, wk = (wg1a, wg1ak) if m < 4 else (wg1b, wg1bk)
            psg, pkg = proj(wt, wk, m % 4, c0, TT)
            sg, sgk = tmpA.next()
            ob, ok = ob32.next()
            P.act(lambda e: e.activation(out=sg[:, 0:TT], in_=psg[:, :], func=AF.Sigmoid), r=[pkg], w=[sgk])
            P.dve(lambda e: e.tensor_tensor(out=ob[:], in0=psy[:, :], in1=sg[:, 0:TT], op=ALU.mult), r=[pky, sgk], w=[ok])
            P.dma("sp", lambda e, m=m: e.dma_start(out=o_mb[m * 128:(m + 1) * 128, tt * TT:(tt + 1) * TT], in_=ob[:]), r=[ok])

    for (u_main, u_rot, nun, ty, dst) in ((U_CQ, U_CQR, 4, 0, o_qd), (U_CK, U_CKR, 4, 0, o_kd)):
        w1, w1k = loadw(u_main)
        w2, w2k = loadw(u_rot)
        for tt in range(NT):
            c0 = H + tt * TT
            make_tables(tt, [0])
            for u in range(nun):
                p1, p1k = proj(w1, w1k, u, c0, TT)
                p2, p2k = proj(w2, w2k, u, c0, TT)
                rope_out(p1, p1k, p2, p2k, ty, dst[u * 128:(u + 1) * 128, tt * TT:(tt + 1) * TT])
    w1, w1k = loadw(U_IQ)
    w2, w2k = loadw(U_IK)
    for tt in range(NT):
        c0 = H + tt * TT
        make_tables(tt, [1, 2])
        for u in range(2):
            p1, p1k = proj(w1, w1k, u, c0, TT)
            p2, p2k = proj(w1, w1k, 2 + u, c0, TT)
            rope_out(p1, p1k, p2, p2k, 1, o_qi[u * 128:(u + 1) * 128, tt * TT:(tt + 1) * TT])
        p1, p1k = proj(w2, w2k, 0, c0, TT)
        p2, p2k = proj(w2, w2k, 1, c0, TT)
        rope_out(p1, p1k, p2, p2k, 1, o_ki[:, tt * TT:(tt + 1) * TT])
        p1, p1k = proj(w2, w2k, 2, c0, TT)
        p2, p2k = proj(w2, w2k, 3, c0, TT)
        rope_out(p1, p1k, p2, p2k, 2, o_kr[:, tt * TT:(tt + 1) * TT])

    w1, w1k = loadw(U_MQ)
    w2, w2k = loadw(U_MKV + 1 - 1)
    lat = hc
    latn = hs
    vaug = Ring(P, 3, [128, 8, 65], BF16, "vaug")
    for i in range(3):
        P.pool(lambda e, i=i: e.memset(vaug.t[i][:], 1.0), w=[vaug.k[i]])

    def rmsnorm_lat(tt, wt, wk, units, nrm_col0, n_feat):
        c0 = H + tt * TT
        L, lk = lat[0], "hc0"
        LN_, lnk = latn[0], "hs0"
        SQ, sqk = sq[0], "sq0"
        nch = len(units)
        for ci, u in enumerate(units):
            p, pk = proj(wt, wk, u, c0, TT)
            P.act(lambda e, ci=ci: e.copy(out=L[:, ci, :], in_=p[:, :]), r=[pk], w=[lk])
            P.act(lambda e, ci=ci: e.activation(out=SQ[:, ci, :], in_=p[:, :], func=AF.Square), r=[pk], w=[sqk])
        pss, pssk = newps()
        for ci in range(nch):
            P.pe(lambda e, ci=ci: e.matmul(pss[:, :], lhsT=ones_sb[:], rhs=SQ[:, ci, :], start=(ci == 0), stop=(ci == nch - 1)),
                 r=["ones", sqk], w=[pssk])
        rstd, rstdk = tmpC.next()
        P.dve(lambda e: e.tensor_scalar(out=rstd[:, 0:TT], in0=pss[:, :], scalar1=1.0 / n_feat, scalar2=EPS, op0=ALU.mult, op1=ALU.add),
              r=[pssk], w=[rstdk])
        P.act(lambda e: e.sqrt(out=rstd[:, 0:TT], in_=rstd[:, 0:TT]), r=[rstdk], w=[rstdk])
        P.dve(lambda e: e.reciprocal(out=rstd[:, 0:TT], in_=rstd[:, 0:TT]), r=[rstdk], w=[rstdk])
        for ci in range(nch):
            P.dve(lambda e, ci=ci: e.scalar_tensor_tensor(out=LN_[:, ci, :], in0=L[:, ci, :], scalar=pvec_sb[:, nrm_col0 + ci:nrm_col0 + ci + 1],
                                                          in1=rstd[:, 0:TT], op0=ALU.mult, op1=ALU.mult), r=[lk, "pvec", rstdk], w=[lnk])
        return LN_, lnk

    for tt in range(NT):
        make_tables(tt, [2])
        LN_, lnk = rmsnorm_lat(tt, w1, w1k, [0, 1, 2], 16, 384.0)
        for m in range(4):
            ps, pk = newps()
            for k in range(3):
                P.pe(lambda e, k=k: e.matmul(ps[:, :], lhsT=wuqn_sb[:, k, m * 128:(m + 1) * 128], rhs=LN_[:, k, :],
                                             start=(k == 0), stop=(k == 2)), r=["wuqn", lnk], w=[pk])
            ob, ok = ob16.next()
            P.act(lambda e: e.copy(out=ob[:], in_=ps[:, :]), r=[pk], w=[ok])
            P.dma("sp", lambda e, m=m: e.dma_start(out=o_qn[m * 128:(m + 1) * 128, tt * TT:(tt + 1) * TT], in_=ob[:]), r=[ok])
        for m in range(2):
            ps1, pk1 = newps()
            ps2, pk2 = newps()
            for k in range(3):
                P.pe(lambda e, k=k: e.matmul(ps1[:, :], lhsT=wuqr_sb[:, k, m * 128:(m + 1) * 128], rhs=LN_[:, k, :],
                                             start=(k == 0), stop=(k == 2)), r=["wuqr", lnk], w=[pk1])
            for k in range(3):
                P.pe(lambda e, k=k: e.matmul(ps2[:, :], lhsT=wuqr_sb[:, k, 256 + m * 128:256 + (m + 1) * 128], rhs=LN_[:, k, :],
                                             start=(k == 0), stop=(k == 2)), r=["wuqr", lnk], w=[pk2])
            rope_out(ps1, pk1, ps2, pk2, 2, o_qr[m * 128:(m + 1) * 128, tt * TT:(tt + 1) * TT])
    for tt in range(NT):
        LN_, lnk = rmsnorm_lat(tt, w2, w2k, [0, 1], 19, 256.0)
        for m in range(4):
            ps, pk = newps()
            for k in range(2):
                P.pe(lambda e, k=k: e.matmul(ps[:, :], lhsT=wuk_sb[:, k, m * 128:(m + 1) * 128], rhs=LN_[:, k, :],
                                             start=(k == 0), stop=(k == 1)), r=["wuk", lnk], w=[pk])
            ob, ok = ob16.next()
            P.act(lambda e: e.copy(out=ob[:], in_=ps[:, :]), r=[pk], w=[ok])
            P.dma("sp", lambda e, m=m: e.dma_start(out=o_kn[m * 128:(m + 1) * 128, tt * TT:(tt + 1) * TT], in_=ob[:]), r=[ok])
        for s in range(4):
            ps, pk = newps()
            for k in range(2):
                P.pe(lambda e, k=k: e.matmul(ps[:, :], lhsT=LN_[:, k, s * 128:(s + 1) * 128], rhs=wuv_sb[:, k, :],
                                             start=(k == 0), stop=(k == 1)), r=["wuv", lnk], w=[pk])
            va, vak = vaug.next()
            P.act(lambda e: e.copy(out=va[:, :, 0:64], in_=ps[:, :].rearrange("p (h d) -> p h d", d=64)), r=[pk], w=[vak])
            r0 = tt * TT + s * 128
            P.dma("sp", lambda e, r0=r0: e.dma_start(out=o_vm[r0:r0 + 128, :], in_=va[:].rearrange("p h d -> p (h d)")), r=[vak])

    for (u0, dst) in ((U_G2, o_g2), (U_G3, o_g3)):
        wa, wak = loadw(u0)
        wb, wbk = loadw(u0 + 4)
        for tt in range(NT):
            c0 = H + tt * TT
            for m in range(8):
                wt, wk = (wa, wak) if m < 4 else (wb, wbk)
                psg, pkg = proj(wt, wk, m % 4, c0, TT)
                ob, ok = ob16.next()
                P.act(lambda e: e.activation(out=ob[:], in_=psg[:, :], func=AF.Sigmoid), r=[pkg], w=[ok])
                P.dma("sp", lambda e, m=m: e.dma_start(out=dst[m * 128:(m + 1) * 128, tt * TT:(tt + 1) * TT], in_=ob[:]), r=[ok])

    wv, wvk = loadw(U_CV)
    ww, wwk = loadw(U_IW)
    iwb = Ring(P, 3, [128, 8], F32, "iwb")
    for s in range(T // 128):
        c0 = H + s * 128
        ps, pk = newps()
        for k in range(8):
            P.pe(lambda e, k=k: e.matmul(ps[:, :], lhsT=xb[:, k, c0:c0 + 128], rhs=wv[:, k, :], start=(k == 0), stop=(k == 7)),
                 r=["xb", wvk], w=[pk])
        va, vak = vaug.next()
        P.act(lambda e: e.copy(out=va[:, :, 0:64], in_=ps[:, :].rearrange("p (h d) -> p h d", d=64)), r=[pk], w=[vak])
        P.dma("sp", lambda e: e.dma_start(out=o_vd[s * 128:(s + 1) * 128, :], in_=va[:].rearrange("p h d -> p (h d)")), r=[vak])
        ps2, pk2 = newps()
        for k in range(8):
            P.pe(lambda e, k=k: e.matmul(ps2[:, 0:8], lhsT=xb[:, k, c0:c0 + 128], rhs=ww[:, k, 0:8], start=(k == 0), stop=(k == 7)),
                 r=["xb", wwk], w=[pk2])
        ib, ibk = iwb.next()
        P.act(lambda e: e.copy(out=ib[:], in_=ps2[:, 0:8]), r=[pk2], w=[ibk])
        P.dma("sp", lambda e: e.dma_start(out=o_iw[s * 128:(s + 1) * 128, :], in_=ib[:]), r=[ibk])
    nc = P.finish()
    return nc, P.stats


def prep_w1(inp, l):
    w = inp["w_in"][l]
    cols = []
    cols.append(np.arange(O_POOL, O_POOL + 512))
    cols.append(np.arange(O_CA, O_CA + 512))
    cols.append(np.arange(O_CG, O_CG + 512))
    rp = rotperm(8, 64, 16)
    cols.append(O_CQ + np.arange(512)); cols.append(O_CQ + rp)
    cols.append(O_CK + np.arange(512)); cols.append(O_CK + rp)
    rpi = rotperm(8, 32, 8)
    cols.append(O_IQ + np.arange(256)); cols.append(O_IQ + rpi)
    r1 = rotperm(1, 32, 8)
    cols.append(np.concatenate([O_IK + np.arange(32)] * 4)); cols.append(np.concatenate([O_IK + r1] * 4))
    r2 = rotperm(1, 32, 32)
    cols.append(np.concatenate([O_MKR + np.arange(32)] * 4)); cols.append(np.concatenate([O_MKR + r2] * 4))
    cols.append(O_MQ + np.arange(384)); cols.append(O_MKV + np.arange(256))
    cols.append(O_G + np.arange(4096))
    cols.append(O_CV + np.arange(512))
    cols.append(np.concatenate([O_IW + np.arange(8)] * 16))
    idx = np.concatenate(cols)
    assert idx.shape[0] == NU * 128, idx.shape
    return np.ascontiguousarray(w[:, idx])


def fm(v, n):
    return np.ascontiguousarray(v.reshape(n, 128).T)


def prep_p1(inp, l, xs):
    W1 = prep_w1(inp, l)
    pvec = np.zeros((128, 32), np.float32)
    pvec[:, 0:4] = fm(inp["pool_scale"][l], 4)
    pvec[:, 4:8] = fm(inp["conv_b"][l], 4)
    pvec[:, 8:12] = fm(inp["conv_ln_g"][l], 4)
    pvec[:, 12:16] = fm(inp["conv_ln_b"][l], 4)
    pvec[:, 16:19] = fm(inp["mla_q_norm"][l], 3)
    pvec[:, 19:21] = fm(inp["mla_kv_norm"][l], 2)
    poolw = np.ascontiguousarray(inp["pool_w"][l].transpose(1, 0, 2).reshape(128, 512))
    convw = np.ascontiguousarray(inp["conv_w"][l].T.reshape(4, 128, 31).transpose(1, 0, 2).reshape(128, 124))
    wuq = inp["mla_wuq"][l]
    hn = np.concatenate([96 * h + np.arange(64) for h in range(8)])
    hr = np.concatenate([96 * h + 64 + np.arange(32) for h in range(8)])
    rp = rotperm(8, 32, 32)
    wuqn = np.ascontiguousarray(wuq[:, hn])
    wuqr = np.ascontiguousarray(np.concatenate([wuq[:, hr], wuq[:, hr[rp]]], axis=1))
    cst = rope_consts()
    maps = []
    for c in range(8):
        b, half = c // 2, c % 2
        x = inp["x_cur"][b]
        s0 = half * T
        xh = np.zeros((H + T, 1024), np.float32)
        xh[H:] = x[s0:s0 + T]
        if half:
            xh[:H] = x[s0 - H:s0]
        pc = np.ones((128, 4, 16), np.float32)
        if half == 0:
            for g, wv in enumerate((2, 4, 8, 16)):
                pc[:, g, :] = wv / np.minimum(np.arange(16) + 1, wv).astype(np.float32)
        maps.append(dict(xT=np.ascontiguousarray(xh.T), W=W1, pos=inp["positions"][b:b + 1, s0:s0 + T].astype(np.int32),
                         cst=cst, pcorr=pc.reshape(128, 64), poolw=poolw, pvec=pvec, poolout=inp["pool_out"][l],
                         convw=convw, convout=inp["conv_out"][l], wuqn=wuqn, wuqr=wuqr,
                         wuk=inp["mla_wuk"][l], wuv=inp["mla_wuv"][l]))
    return maps


S = 8192
NSLOT = 4
MLA_SCALE = 96 ** -0.5


def slot_tiles(j):
    return 8 * j + 8, 64 - 8 * j


def core_groups(half):
    return [((2 * j + half), (15 - 2 * j - half)) for j in range(NSLOT)]


def build_mla():
    P = Prog()
    qn = P.dram_in("qn", [512, 4096], BF16)
    qr = P.dram_in("qr", [512, 4096], BF16)
    kn = P.dram_in("kn", [512, S], BF16)
    kr = P.dram_in("kr", [128, S], BF16)
    vm = P.dram_in("vm", [S, 520], BF16)
    msk = P.dram_in("msk", [2 * 8 * 128, 512], BF16)
    idn = P.dram_in("idn", [128, 128], BF16)
    o_o = P.dram_out("o", [512, 4096], BF16)

    kn_sb = P.sb([128, 4, S], BF16, "kn_sb")
    kr_sb = P.sb([128, S], BF16, "kr_sb")
    vm_sb = P.sb([128, 64, 520], BF16, "vm_sb")
    msk_sb = P.sb([128, 16, 512], BF16, "msk_sb")
    idn_sb = P.sb([128, 128], BF16, "idn_sb")
    ones_sb = P.sb([128, 64], F32, "ones_sb")
    qn_r = Ring(P, 2, [128, 4, 512], BF16, "qn_r")
    qr_r = Ring(P, 2, [128, 4, 512], BF16, "qr_r")
    pT_r = Ring(P, 3, [128, 512], BF16, "pT")
    osb_r = Ring(P, 2, [128, 512], F32, "osb")
    rcp_r = Ring(P, 2, [64, 512], F32, "rcp")
    on_r = Ring(P, 2, [64, 512], BF16, "on")

    for c in range(4):
        P.dma("sp" if c % 2 else "act", lambda e, c=c: e.dma_start(out=kn_sb[:, c, :], in_=kn[c * 128:(c + 1) * 128, :]), w=["kn"])
    P.dma("sp", lambda e: e.dma_start(out=kr_sb[:], in_=kr), w=["kr"])
    for q4 in range(4):
        P.dma("pool", lambda e, q4=q4: e.dma_start(out=vm_sb[:, q4 * 16:(q4 + 1) * 16, :],
                                                   in_=vm[q4 * 2048:(q4 + 1) * 2048, :].rearrange("(t p) n -> p t n", p=128)), w=["vm"])
    P.dma("sp", lambda e: e.dma_start(out=msk_sb[:], in_=msk.rearrange("(t p) n -> p t n", p=128)), w=["msk"])
    P.dma("sp", lambda e: e.dma_start(out=idn_sb[:], in_=idn), w=["idn"])
    P.dve(lambda e: e.memset(ones_sb[:], 1.0), w=["ones"])

    PSQ = [P.ps([128, 512], F32, name=f"psq{i}") for i in range(4)]
    PSO = [P.ps([128, 512], F32, name=f"pso{i}") for i in range(2)]
    PSB = [P.ps([128, 512], F32, name=f"psbb{i}") for i in range(2)]
    ctr = dict(q=0, o=0, b=0)

    for j in range(NSLOT):
        for part, ntiles in enumerate(slot_tiles(j)):
            col0 = (2 * j + part) * 512
            qnt, qnk = qn_r.next()
            qrt, qrk = qr_r.next()
            P.dma("sp", lambda e: e.dma_start(out=qnt[:], in_=qn[:, col0:col0 + 512].rearrange("(c p) n -> p c n", p=128)), w=[qnk])
            P.dma("sp", lambda e: e.dma_start(out=qrt[:], in_=qr[:, col0:col0 + 512].rearrange("(c p) n -> p c n", p=128)), w=[qrk])
            for h in range(8):
                ch, b0 = h // 2, 64 * (h % 2)
                rc, rb = h // 2, 32 * (h % 2)
                po = PSO[ctr["o"] % 2]
                pok = f"pso{ctr['o'] % 2}"
                ctr["o"] += 1
                def qk(kt):
                    ps = PSQ[ctr["q"] % 4]
                    psk = f"psq{ctr['q'] % 4}"
                    ctr["q"] += 1
                    mi = kt - (ntiles - 8)
                    ks = slice(kt * 128, (kt + 1) * 128)
                    P.pe(lambda e: e.matmul(ps[:, :], lhsT=kn_sb[b0:b0 + 64, ch, ks], rhs=qnt[b0:b0 + 64, ch, :], start=True, stop=False),
                         r=["kn", qnk], w=[psk])
                    P.pe(lambda e: e.matmul(ps[:, :], lhsT=kr_sb[rb:rb + 32, ks], rhs=qrt[rb:rb + 32, rc, :], start=False, stop=(mi < 0)),
                         r=["kr", qrk], w=[psk])
                    if mi >= 0:
                        P.pe(lambda e: e.matmul(ps[:, :], lhsT=idn_sb[:], rhs=msk_sb[:, part * 8 + mi, :], start=False, stop=True),
                             r=["idn", "msk"], w=[psk])
                    return ps, psk
                pend = qk(0)
                for kt in range(ntiles):
                    ps, psk = pend
                    if kt + 1 < ntiles:
                        pend = qk(kt + 1)
                    pT, pTk = pT_r.next()
                    P.act(lambda e: e.activation(out=pT[:], in_=ps[:, :], func=AF.Exp, scale=MLA_SCALE), r=[psk], w=[pTk])
                    P.pe(lambda e: e.matmul(po[0:65, :], lhsT=vm_sb[:, kt, h * 65:(h + 1) * 65], rhs=pT[:], start=(kt == 0), stop=(kt == ntiles - 1)),
                         r=["vm", pTk], w=[pok])
                osb, osk = osb_r.next()
                P.act(lambda e: e.copy(out=osb[0:65, :], in_=po[0:65, :]), r=[pok], w=[osk])
                pb = PSB[ctr["b"] % 2]
                pbk = f"psbb{ctr['b'] % 2}"
                ctr["b"] += 1
                P.pe(lambda e: e.matmul(pb[0:64, :], lhsT=ones_sb[64:65, 0:64], rhs=osb[64:65, :], start=True, stop=True),
                     r=["ones", osk], w=[pbk])
                rcp, rck = rcp_r.next()
                on, onk = on_r.next()
                P.dve(lambda e: e.reciprocal(out=rcp[:], in_=pb[0:64, :]), r=[pbk], w=[rck])
                P.dve(lambda e: e.tensor_tensor(out=on[:], in0=osb[0:64, :], in1=rcp[:], op=ALU.mult), r=[osk, rck], w=[onk])
                P.dma("sp", lambda e: e.dma_start(out=o_o[h * 64:(h + 1) * 64, col0:col0 + 512], in_=on[:]), r=[onk])
    nc = P.finish()
    return nc, P.stats


def make_masks(half):
    NEG = -30000.0
    kk = np.arange(128)[:, None]
    qq = np.arange(512)[None, :]
    D = [np.where(j * 128 + kk <= qq, 0.0, NEG).astype(np.float32) for j in range(4)]
    Z = np.zeros((128, 512), np.float32)
    F = np.full((128, 512), NEG, np.float32)
    dz = [D[0], D[1], D[2], D[3], F, F, F, F]
    zd = [Z, Z, Z, Z, D[0], D[1], D[2], D[3]]
    lo, hi = (dz, zd) if half == 0 else (zd, dz)
    return np.stack(lo + hi).reshape(16 * 128, 512).astype(NPBF)


def qcols(half):
    cols = []
    for (lo, hi) in core_groups(half):
        cols.append(np.arange(lo * 512, (lo + 1) * 512))
        cols.append(np.arange(hi * 512, (hi + 1) * 512))
    return np.concatenate(cols)


def relayout_qr(qr):
    out = np.zeros((512, qr.shape[1]), qr.dtype)
    for h in range(8):
        r0 = (h // 2) * 128 + (h % 2) * 32
        out[r0:r0 + 32] = qr[h * 32:(h + 1) * 32]
    return out


DSA_SCALE = 64 ** -0.5
NIT = 26
NEG_S = -1.0e9


def build_dsa():
    P = Prog()
    qd = P.dram_in("qd", [512, 4096], BF16)
    kd = P.dram_in("kd", [512, S], BF16)
    vd = P.dram_in("vd", [S, 520], BF16)
    qi = P.dram_in("qi", [512, 4096], BF16)
    ki = P.dram_in("ki", [128, S], BF16)
    iw = P.dram_in("iw", [4096, 8], F32)
    cmsk = P.dram_in("cmsk", [2 * 4 * 128, 1024], F32)
    idn = P.dram_in("idn", [128, 128], BF16)
    o_o = P.dram_out("o", [512, 4096], BF16)
    mscr = [[P.dram_tmp(f"mscr{j}_{p}", [512, slot_tiles(j)[p] * 128], BF16) for p in range(2)] for j in range(NSLOT)]

    PSQ = [P.ps([128, 512], F32, name=f"psq{i}") for i in range(4)]
    PSO = [P.ps([128, 512], F32, name=f"pso{i}") for i in range(2)]
    PSB = [P.ps([128, 512], F32, name=f"psbb{i}") for i in range(2)]
    ctr = dict(q=0, o=0, b=0)

    st1 = ExitStack()
    def sb1(shape, dt, name):
        return st1.enter_context(P.nc.sbuf_tensor(name, list(shape), dt))
    ki_sb = sb1([128, S], BF16, "ki_sb")
    qi_sb = [sb1([128, 4, 512], BF16, f"qi_sb{i}") for i in range(2)]
    iw_sb = [sb1([128, 4, 8], F32, f"iw_sb{i}") for i in range(2)]
    cm_sb = sb1([128, 8, 1024], F32, "cm_sb")
    sc = [sb1([128, S], F32, f"sc{i}") for i in range(2)]
    junk = sb1([128, S], BF16, "junk")
    mbo = [sb1([128, S], BF16, f"mbo{i}") for i in range(2)]
    rl = [sb1([128, 512], F32, f"rl{i}") for i in range(3)]
    sm = [sb1([128, 16], F32, f"sm{i}") for i in range(2)]
    P.dma("sp", lambda e: e.dma_start(out=ki_sb[:], in_=ki), w=["ki"])
    P.dma("act", lambda e: e.dma_start(out=cm_sb[:], in_=cmsk.rearrange("(t p) n -> p t n", p=128)), w=["cm"])
    gi = 0
    rli = 0
    for j in range(NSLOT):
        for part, ntiles in enumerate(slot_tiles(j)):
            col0 = (2 * j + part) * 512
            L = ntiles * 128
            qit, qik = qi_sb[gi % 2], f"qi{gi % 2}"
            iwt, iwk = iw_sb[gi % 2], f"iw{gi % 2}"
            gi += 1
            P.dma("sp", lambda e: e.dma_start(out=qit[:], in_=qi[:, col0:col0 + 512].rearrange("(c p) n -> p c n", p=128)), w=[qik])
            P.dma("sp", lambda e: e.dma_start(out=iwt[:], in_=iw[col0:col0 + 512, :].rearrange("(r p) h -> p r h", p=128)), w=[iwk])
            for r in range(4):
                bi = (gi * 4 + r) % 2
                s_, sk = sc[bi], f"sc{bi}"
                m_, mk = mbo[bi], f"mbo{bi}"
                z_, zk = sm[bi], f"sm{bi}"
                for kc in range(L // 512):
                    for h in range(8):
                        ps = PSQ[ctr["q"] % 4]
                        psk = f"psq{ctr['q'] % 4}"
                        ctr["q"] += 1
                        rc, rb = h // 2, 32 * (h % 2)
                        P.pe(lambda e: e.matmul(ps[:, :], lhsT=qit[rb:rb + 32, rc, r * 128:(r + 1) * 128],
                                                rhs=ki_sb[rb:rb + 32, kc * 512:(kc + 1) * 512], start=True, stop=True),
                             r=[qik, "ki"], w=[psk])
                        rt, rk = rl[rli % 3], f"rl{rli % 3}"
                        rli += 1
                        P.act(lambda e: e.activation(out=rt[:], in_=ps[:, :], func=AF.Relu), r=[psk], w=[rk])
                        if h == 0:
                            P.dve(lambda e: e.tensor_scalar(out=s_[:, kc * 512:(kc + 1) * 512], in0=rt[:], scalar1=iwt[:, r, 0:1],
                                                            scalar2=None, op0=ALU.mult), r=[rk, iwk], w=[sk])
                        else:
                            P.dve(lambda e: e.scalar_tensor_tensor(out=s_[:, kc * 512:(kc + 1) * 512], in0=rt[:], scalar=iwt[:, r, h:h + 1],
                                                                   in1=s_[:, kc * 512:(kc + 1) * 512], op0=ALU.mult, op1=ALU.add),
                                  r=[rk, iwk, sk], w=[sk])
                P.dve(lambda e: e.tensor_reduce(out=z_[:, 0:1], in_=s_[:, 0:L], axis=AX.X, op=ALU.max), r=[sk], w=[zk])
                P.dve(lambda e: e.tensor_reduce(out=z_[:, 7:8], in_=s_[:, 0:L], axis=AX.X, op=ALU.min), r=[sk], w=[zk])
                P.dve(lambda e: e.scalar_tensor_tensor(out=z_[:, 0:1], in0=z_[:, 7:8], scalar=-1.0, in1=z_[:, 0:1], op0=ALU.mult, op1=ALU.max),
                      r=[zk], w=[zk])
                P.dve(lambda e: e.tensor_scalar(out=z_[:, 0:1], in0=z_[:, 0:1], scalar1=1.0, scalar2=None, op0=ALU.add), r=[zk], w=[zk])
                P.dve(lambda e: e.tensor_scalar(out=z_[:, 1:2], in0=z_[:, 0:1], scalar1=2.0, scalar2=None, op0=ALU.mult), r=[zk], w=[zk])
                P.dve(lambda e: e.tensor_scalar(out=z_[:, 2:3], in0=z_[:, 0:1], scalar1=-1.0, scalar2=None, op0=ALU.mult), r=[zk], w=[zk])
                P.dve(lambda e: e.tensor_tensor(out=s_[:, L - 1024:L], in0=s_[:, L - 1024:L], in1=cm_sb[:, part * 4 + r, :], op=ALU.add),
                      r=[sk, "cm"], w=[sk])
                lo_c, lo_n = 2, 3
                for it in range(1, NIT + 1):
                    f = 2.0 ** (-it)
                    P.dve(lambda e: e.scalar_tensor_tensor(out=z_[:, 4:5], in0=z_[:, 1:2], scalar=f, in1=z_[:, lo_c:lo_c + 1],
                                                           op0=ALU.mult, op1=ALU.add), r=[zk], w=[zk])
                    P.dve(lambda e: e.memset(z_[:, 5:6], 0.0), w=[zk])
                    P.dve(lambda e: e.tensor_scalar(out=junk[:, 0:L], in0=s_[:, 0:L], scalar1=z_[:, 4:5], scalar2=0.0,
                                                    op0=ALU.is_ge, op1=ALU.add, accum_out=z_[:, 5:6]), r=[sk, zk], w=["junk", zk])
                    P.dve(lambda e: e.tensor_scalar(out=z_[:, 6:7], in0=z_[:, 5:6], scalar1=255.5, scalar2=f, op0=ALU.is_ge, op1=ALU.mult),
                          r=[zk], w=[zk])
                    P.dve(lambda e: e.scalar_tensor_tensor(out=z_[:, lo_n:lo_n + 1], in0=z_[:, 6:7], scalar=z_[:, 1:2], in1=z_[:, lo_c:lo_c + 1],
                                                           op0=ALU.mult, op1=ALU.add), r=[zk], w=[zk])
                    lo_c, lo_n = lo_n, lo_c
                P.dve(lambda e: e.tensor_scalar(out=m_[:, 0:L], in0=s_[:, 0:L], scalar1=z_[:, lo_c:lo_c + 1], scalar2=-30000.0,
                                                op0=ALU.is_lt, op1=ALU.mult), r=[sk, zk], w=[mk])
                P.dma("sp", lambda e: e.dma_start(out=mscr[j][part][r * 128:(r + 1) * 128, :], in_=m_[:, 0:L]), r=[mk], w=[f"mscr{j}_{part}"])
    P.barrier()
    st1.close()

    kd_sb = P.sb([128, 4, S], BF16, "kd_sb")
    vd_sb = P.sb([128, 64, 520], BF16, "vd_sb")
    idn_sb = P.sb([128, 128], BF16, "idn_sb")
    ones_sb = P.sb([128, 64], F32, "ones_sb")
    mb_sb = P.sb([128, 4, S], BF16, "mb_sb")
    qd_r = Ring(P, 1, [128, 4, 512], BF16, "qd_r")
    pT_r = Ring(P, 3, [128, 512], BF16, "pT")
    osb_r = Ring(P, 1, [128, 512], F32, "osb")
    rcp_r = Ring(P, 1, [64, 512], F32, "rcp")
    on_r = Ring(P, 2, [64, 512], BF16, "on")
    for c in range(4):
        P.dma("sp" if c % 2 else "act", lambda e, c=c: e.dma_start(out=kd_sb[:, c, :], in_=kd[c * 128:(c + 1) * 128, :]), w=["kd"])
    for q4 in range(4):
        P.dma("pool", lambda e, q4=q4: e.dma_start(out=vd_sb[:, q4 * 16:(q4 + 1) * 16, :],
                                                   in_=vd[q4 * 2048:(q4 + 1) * 2048, :].rearrange("(t p) n -> p t n", p=128)), w=["vd"])
    P.dma("sp", lambda e: e.dma_start(out=idn_sb[:], in_=idn), w=["idn"])
    P.dve(lambda e: e.memset(ones_sb[:], 1.0), w=["ones"])
    for j in range(NSLOT):
        for part, ntiles in enumerate(slot_tiles(j)):
            col0 = (2 * j + part) * 512
            L = ntiles * 128
            qdt, qdk = qd_r.next()
            P.dma("sp", lambda e: e.dma_start(out=qdt[:], in_=qd[:, col0:col0 + 512].rearrange("(c p) n -> p c n", p=128)), w=[qdk])
            P.dma("act", lambda e: e.dma_start(out=mb_sb[:, :, 0:L], in_=mscr[j][part].rearrange("(r p) n -> p r n", p=128)),
                  r=[f"mscr{j}_{part}"], w=["mb"])
            for h in range(8):
                ch, b0 = h // 2, 64 * (h % 2)
                po = PSO[ctr["o"] % 2]
                pok = f"pso{ctr['o'] % 2}"
                ctr["o"] += 1
                def qk(kt):
                    ps = PSQ[ctr["q"] % 4]
                    psk = f"psq{ctr['q'] % 4}"
                    ctr["q"] += 1
                    ks = slice(kt * 128, (kt + 1) * 128)
                    P.pe(lambda e: e.matmul(ps[:, :], lhsT=kd_sb[b0:b0 + 64, ch, ks], rhs=qdt[b0:b0 + 64, ch, :], start=True, stop=False),
                         r=["kd", qdk], w=[psk])
                    for r in range(4):
                        P.pe(lambda e, r=r: e.matmul(ps[:, r * 128:(r + 1) * 128], lhsT=mb_sb[:, r, ks], rhs=idn_sb[:], start=False, stop=(r == 3)),
                             r=["mb", "idn"], w=[psk])
                    return ps, psk
                pend = qk(0)
                for kt in range(ntiles):
                    ps, psk = pend
                    if kt + 1 < ntiles:
                        pend = qk(kt + 1)
                    pT, pTk = pT_r.next()
                    P.act(lambda e: e.activation(out=pT[:], in_=ps[:, :], func=AF.Exp, scale=DSA_SCALE), r=[psk], w=[pTk])
                    P.pe(lambda e: e.matmul(po[0:65, :], lhsT=vd_sb[:, kt, h * 65:(h + 1) * 65], rhs=pT[:], start=(kt == 0), stop=(kt == ntiles - 1)),
                         r=["vd", pTk], w=[pok])
                osb, osk = osb_r.next()
                P.act(lambda e: e.copy(out=osb[0:65, :], in_=po[0:65, :]), r=[pok], w=[osk])
                pb = PSB[ctr["b"] % 2]
                pbk = f"psbb{ctr['b'] % 2}"
                ctr["b"] += 1
                P.pe(lambda e: e.matmul(pb[0:64, :], lhsT=ones_sb[64:65, 0:64], rhs=osb[64:65, :], start=True, stop=True),
                     r=["ones", osk], w=[pbk])
                rcp, rck = rcp_r.next()
                on, onk = on_r.next()
                P.dve(lambda e: e.reciprocal(out=rcp[:], in_=pb[0:64, :]), r=[pbk], w=[rck])
                P.dve(lambda e: e.tensor_tensor(out=on[:], in0=osb[0:64, :], in1=rcp[:], op=ALU.mult), r=[osk, rck], w=[onk])
                P.dma("sp", lambda e: e.dma_start(out=o_o[h * 64:(h + 1) * 64, col0:col0 + 512], in_=on[:]), r=[onk])
    nc = P.finish()
    return nc, P.stats


def make_cmask(half):
    qq = np.arange(512)[:, None]
    out = np.zeros((2, 4, 128, 1024), np.float32)
    for part in range(2):
        dz = (part == 0) == (half == 0)
        kk = np.arange(1024)[None, :]
        if dz:
            keyrel = kk
        else:
            keyrel = kk - 512
        m = np.where(keyrel <= qq, 0.0, NEG_S).astype(np.float32)
        out[part] = m.reshape(4, 128, 1024)
    return out.reshape(8 * 128, 1024)


NT3 = 16384
NEXP = 32
STOP = 0
ALPHA = 4.0 ** 0.25
EPS3 = 1e-5
SWA = 1.702
LIM = 7.0


def build_p3():
    P = Prog()
    ma = P.dram_in("ma", [1024, NT3], F32)
    mb = P.dram_in("mb", [1024, NT3], F32)
    g2 = P.dram_in("g2", [1024, NT3], BF16)
    g3 = P.dram_in("g3", [1024, NT3], BF16)
    oc = P.dram_in("oc", [512, NT3], BF16)
    od = P.dram_in("od", [512, NT3], BF16)
    x = P.dram_in("x", [NT3, 1024], F32)
    dsaout = P.dram_in("dsaout", [512, 1024], F32)
    mlaout = P.dram_in("mlaout", [512, 1024], F32)
    wo = P.dram_in("wo", [1024, 1024], F32)
    lnv = P.dram_in("lnv", [1, 4096], F32)
    rw = P.dram_in("rw", [1024, 32], F32)
    rb = P.dram_in("rb", [1, 32], F32)
    wgu = P.dram_in("wgu", [NEXP * 1024, 2048], F32)
    bgu = P.dram_in("bgu", [NEXP * 128, 16], F32)
    wd = P.dram_in("wd", [NEXP * 1024, 1024], F32)
    bd = P.dram_in("bd", [NEXP, 1024], F32)
    idn = P.dram_in("idn", [128, 128], F32)
    o_x2 = P.dram_out("x2", [NT3, 1024], F32)
    x1_d = P.dram_tmp("x1_d", [NT3, 1024], F32)
    x1T_d = P.dram_tmp("x1T_d", [1024, NT3], BF16)
    G_d = P.dram_tmp("G_d", [NT3, 32], F32)
    wgu_bf = P.dram_tmp("wgu_bf", [NEXP * 1024, 2048], BF16)
    wd_bf = P.dram_tmp("wd_bf", [NEXP * 1024, 1024], BF16)

    PS = [P.ps([128, 512], F32, name=f"psb{i}") for i in range(8)]
    pctr = [0]

    def chk(n):
        if STOP == n:
            P.dead = True

    def newps():
        i = pctr[0] % 8
        pctr[0] += 1
        return PS[i], f"ps{i}"

    def layernorm(u, uk, gb, gbk, gi, out, outk, tmp):
        st, stk = tmp["st"]
        c, ck = tmp["c"]
        jk, jkk = tmp["junk"]
        P.dve(lambda e: e.tensor_reduce(out=st[:, 0:1], in_=u, axis=AX.X, op=ALU.add), r=[uk], w=[stk])
        P.dve(lambda e: e.tensor_scalar(out=st[:, 1:2], in0=st[:, 0:1], scalar1=-1.0 / 1024, scalar2=None, op0=ALU.mult), r=[stk], w=[stk])
        P.dve(lambda e: e.tensor_scalar(out=c[:], in0=u, scalar1=st[:, 1:2], scalar2=None, op0=ALU.add), r=[uk, stk], w=[ck])
        P.act(lambda e: e.activation(out=jk[:], in_=c[:], func=AF.Square), r=[ck], w=[jkk])
        P.dve(lambda e: e.tensor_reduce(out=st[:, 2:3], in_=jk[:], axis=AX.X, op=ALU.add), r=[jkk], w=[stk])
        P.dve(lambda e: e.tensor_scalar(out=st[:, 3:4], in0=st[:, 2:3], scalar1=1.0 / 1024, scalar2=EPS3, op0=ALU.mult, op1=ALU.add),
              r=[stk], w=[stk])
        P.act(lambda e: e.sqrt(out=st[:, 4:5], in_=st[:, 3:4]), r=[stk], w=[stk])
        P.dve(lambda e: e.reciprocal(out=st[:, 5:6], in_=st[:, 4:5]), r=[stk], w=[stk])
        P.dve(lambda e: e.scalar_tensor_tensor(out=jk[:], in0=c[:], scalar=st[:, 5:6], in1=gb[:, gi * 1024:(gi + 1) * 1024], op0=ALU.mult, op1=ALU.mult),
              r=[ck, stk, gbk, jkk], w=[jkk])
        P.pool(lambda e: e.tensor_tensor(out=out, in0=jk[:], in1=gb[:, (gi + 1) * 1024:(gi + 2) * 1024], op=ALU.add), r=[jkk, gbk], w=[outk])

    gb_sb = P.sb([128, 4096], F32, "gb_sb")
    P.dma("sp", lambda e: e.dma_start(out=gb_sb[:], in_=lnv.partition_broadcast(128)), w=["gb"])
    st_r = Ring(P, 2, [128, 8], F32, "st")
    c_r = Ring(P, 1, [128, 1024], F32, "cc")
    jk_r = Ring(P, 1, [128, 1024], F32, "jk")
    u_r = Ring(P, 2, [128, 1024], F32, "u")
    x1s_r = Ring(P, 2, [128, 1024], F32, "x1s")

    def lntmp():
        return dict(st=st_r.next(), c=c_r.next(), junk=jk_r.next())

    stA = ExitStack()

    def sbA(shape, dt, name):
        return stA.enter_context(P.nc.sbuf_tensor(name, list(shape), dt))

    dsaout_sb = sbA([128, 4, 1024], BF16, "dsaout_sb")
    mlaout_sb = sbA([128, 4, 1024], BF16, "mlaout_sb")
    wo_sb = sbA([128, 8, 1024], BF16, "wo_sb")
    rw_sb = sbA([128, 8, 32], F32, "rw_sb")
    rb_sb = sbA([128, 32], F32, "rb_sb")
    idn_sb = sbA([128, 128], F32, "idn_sb")
    ma_sb = sbA([128, 8, 512], F32, "ma_sb")
    mb_sb = sbA([128, 8, 512], F32, "mb_sb")
    g2_sb = sbA([128, 8, 512], BF16, "g2_sb")
    g3_sb = sbA([128, 8, 512], BF16, "g3_sb")
    oc_sb = sbA([128, 4, 512], BF16, "oc_sb")
    od_sb = sbA([128, 4, 512], BF16, "od_sb")
    mg_sb = [sbA([128, 8, 512], BF16, f"mg_sb{i}") for i in range(2)]
    t1_sb = [sbA([128, 512], F32, f"t1_sb{i}") for i in range(2)]
    t2_sb = [sbA([128, 512], F32, f"t2_sb{i}") for i in range(2)]
    t3_sb = [sbA([128, 512], F32, f"t3_sb{i}") for i in range(2)]
    xs_sb = [sbA([128, 1024], F32, f"xs_sb{i}") for i in range(2)]
    xTf_sb = sbA([128, 8, 128], F32, "xTf_sb")
    xTb_sb = [sbA([128, 8, 128], BF16, f"xTb_sb{i}") for i in range(2)]
    rt_sb = [sbA([128, 96], F32, f"rt_sb{i}") for i in range(2)]
    rs_sb = [sbA([128, 16], F32, f"rs_sb{i}") for i in range(2)]
    G_sb = [sbA([128, 32], F32, f"G_sb{i}") for i in range(2)]
    P.dma("pool", lambda e: e.dma_start(out=dsaout_sb[:], in_=dsaout.rearrange("(k p) n -> p k n", p=128)), w=["dsaout"])
    P.dma("pool", lambda e: e.dma_start(out=mlaout_sb[:], in_=mlaout.rearrange("(k p) n -> p k n", p=128)), w=["mlaout"])
    P.dma("pool", lambda e: e.dma_start(out=wo_sb[:], in_=wo.rearrange("(k p) n -> p k n", p=128)), w=["wo"])
    P.dma("sp", lambda e: e.dma_start(out=rw_sb[:], in_=rw.rearrange("(k p) n -> p k n", p=128)), w=["rw"])
    P.dma("sp", lambda e: e.dma_start(out=rb_sb[:], in_=rb.partition_broadcast(128)), w=["rb"])
    P.dma("sp", lambda e: e.dma_start(out=idn_sb[:], in_=idn), w=["idn"])
    chk(1)
    si = 0
    for tt in range(NT3 // 512):
        cs = slice(tt * 512, (tt + 1) * 512)
        P.dma("sp", lambda e: e.dma_start(out=ma_sb[:], in_=ma[:, cs].rearrange("(m p) n -> p m n", p=128)), w=["ma"])
        P.dma("act", lambda e: e.dma_start(out=mb_sb[:], in_=mb[:, cs].rearrange("(m p) n -> p m n", p=128)), w=["mb"])
        P.dma("sp", lambda e: e.dma_start(out=g2_sb[:], in_=g2[:, cs].rearrange("(m p) n -> p m n", p=128)), w=["g2"])
        P.dma("act", lambda e: e.dma_start(out=g3_sb[:], in_=g3[:, cs].rearrange("(m p) n -> p m n", p=128)), w=["g3"])
        P.dma("sp", lambda e: e.dma_start(out=oc_sb[:], in_=oc[:, cs].rearrange("(m p) n -> p m n", p=128)), w=["oc"])
        P.dma("act", lambda e: e.dma_start(out=od_sb[:], in_=od[:, cs].rearrange("(m p) n -> p m n", p=128)), w=["od"])
        mg, mgk = mg_sb[tt % 2], f"mg{tt % 2}"
        for m in range(8):
            pc, pck = newps()
            pd, pdk = newps()
            for k in range(4):
                P.pe(lambda e, k=k: e.matmul(pc[:, :], lhsT=dsaout_sb[:, k, m * 128:(m + 1) * 128], rhs=oc_sb[:, k, :], start=(k == 0), stop=(k == 3)),
                     r=["dsaout", "oc"], w=[pck])
            for k in range(4):
                P.pe(lambda e, k=k: e.matmul(pd[:, :], lhsT=mlaout_sb[:, k, m * 128:(m + 1) * 128], rhs=od_sb[:, k, :], start=(k == 0), stop=(k == 3)),
                     r=["mlaout", "od"], w=[pdk])
            t1, t1k = t1_sb[m % 2], f"t1{m % 2}"
            t2, t2k = t2_sb[m % 2], f"t2{m % 2}"
            t3, t3k = t3_sb[m % 2], f"t3{m % 2}"
            P.dve(lambda e: e.tensor_tensor(out=t1[:], in0=pc[:, :], in1=g2_sb[:, m, :], op=ALU.mult), r=[pck, "g2"], w=[t1k])
            P.dve(lambda e: e.tensor_tensor(out=t2[:], in0=pd[:, :], in1=g3_sb[:, m, :], op=ALU.mult), r=[pdk, "g3"], w=[t2k])
            P.pool(lambda e: e.tensor_tensor(out=t3[:], in0=ma_sb[:, m, :], in1=mb_sb[:, m, :], op=ALU.add), r=["ma", "mb"], w=[t3k])
            P.pool(lambda e: e.tensor_tensor(out=t1[:], in0=t1[:], in1=t2[:], op=ALU.add), r=[t1k, t2k], w=[t1k])
            P.pool(lambda e: e.tensor_tensor(out=mg[:, m, :], in0=t1[:], in1=t3[:], op=ALU.add), r=[t1k, t3k], w=[mgk])
        chk(2)
        for s in range(4):
            tok0 = tt * 512 + s * 128
            xs, xsk = xs_sb[si % 2], f"xs{si % 2}"
            xTb, xTbk = xTb_sb[si % 2], f"xTb{si % 2}"
            rt, rtk = rt_sb[si % 2], f"rt{si % 2}"
            rs, rsk = rs_sb[si % 2], f"rs{si % 2}"
            Gs, Gsk = G_sb[si % 2], f"G{si % 2}"
            si += 1
            P.dma("sp", lambda e: e.dma_start(out=xs[:], in_=x[tok0:tok0 + 128, :]), w=[xsk])
            u, uk = u_r.next()
            for half in range(2):
                pz, pzk = newps()
                for k in range(8):
                    P.pe(lambda e, k=k: e.matmul(pz[:, :], lhsT=mg[:, k, s * 128:(s + 1) * 128], rhs=wo_sb[:, k, half * 512:(half + 1) * 512],
                                                 start=(k == 0), stop=(k == 7)), r=[mgk, "wo"], w=[pzk])
                P.dve(lambda e: e.scalar_tensor_tensor(out=u[:, half * 512:(half + 1) * 512], in0=xs[:, half * 512:(half + 1) * 512], scalar=ALPHA,
                                                       in1=pz[:, :], op0=ALU.mult, op1=ALU.add), r=[xsk, pzk], w=[uk])
            x1s, x1k = x1s_r.next()
            layernorm(u[:], uk, gb_sb, "gb", 0, x1s[:], x1k, lntmp())
            chk(3)
            P.dma("sp", lambda e: e.dma_start(out=x1_d[tok0:tok0 + 128, :], in_=x1s[:]), r=[x1k], w=["x1_d"])
            chk(31)
            for hb in range(2):
                pt, ptk = newps()
                for q in range(4):
                    k = hb * 4 + q
                    P.pe(lambda e, k=k, q=q: e.matmul(pt[:, q * 128:(q + 1) * 128], lhsT=x1s[:, k * 128:(k + 1) * 128], rhs=idn_sb[:], start=True, stop=True),
                         r=[x1k, "idn"], w=[ptk])
                chk(32)
                for q in range(4):
                    k = hb * 4 + q
                    P.act(lambda e, k=k, q=q: e.copy(out=xTf_sb[:, k, :], in_=pt[:, q * 128:(q + 1) * 128]), r=[ptk], w=["xTf"])
            P.dve(lambda e: e.tensor_copy(out=xTb[:], in_=xTf_sb[:]), r=["xTf"], w=[xTbk])
            chk(33)
            for k in range(8):
                P.dma("act" if k % 2 else "sp", lambda e, k=k: e.dma_start(out=x1T_d[k * 128:(k + 1) * 128, tok0:tok0 + 128], in_=xTb[:, k, :]), r=[xTbk], w=["x1T_d"])
            chk(4)
            pr, prk = newps()
            for k in range(8):
                P.pe(lambda e, k=k: e.matmul(pr[:, 0:32], lhsT=xTf_sb[:, k, :], rhs=rw_sb[:, k, :], start=(k == 0), stop=(k == 7)),
                     r=["xTf", "rw"], w=[prk])
            P.dve(lambda e: e.tensor_tensor(out=rt[:, 0:32], in0=pr[:, 0:32], in1=rb_sb[:], op=ALU.add), r=[prk, "rb"], w=[rtk])
            P.dve(lambda e: e.max(out=rs[:, 0:8], in_=rt[:, 0:32]), r=[rtk], w=[rsk])
            P.dve(lambda e: e.tensor_scalar(out=rs[:, 8:9], in0=rs[:, 0:1], scalar1=-1.0, scalar2=None, op0=ALU.mult), r=[rsk], w=[rsk])
            P.act(lambda e: e.activation(out=rt[:, 32:64], in_=rt[:, 0:32], func=AF.Exp, bias=rs[:, 8:9], scale=1.0), r=[rtk, rsk], w=[rtk])
            P.dve(lambda e: e.scalar_tensor_tensor(out=rt[:, 64:96], in0=rt[:, 0:32], scalar=rs[:, 3:4], in1=rt[:, 32:64], op0=ALU.is_ge, op1=ALU.mult),
                  r=[rtk, rsk], w=[rtk])
            P.dve(lambda e: e.tensor_reduce(out=rs[:, 9:10], in_=rt[:, 64:96], axis=AX.X, op=ALU.add), r=[rtk], w=[rsk])
            P.dve(lambda e: e.reciprocal(out=rs[:, 10:11], in_=rs[:, 9:10]), r=[rsk], w=[rsk])
            P.dve(lambda e: e.tensor_scalar(out=Gs[:], in0=rt[:, 64:96], scalar1=rs[:, 10:11], scalar2=None, op0=ALU.mult), r=[rtk, rsk], w=[Gsk])
            P.dma("sp", lambda e: e.dma_start(out=G_d[tok0:tok0 + 128, :], in_=Gs[:]), r=[Gsk], w=["G_d"])
            chk(5)
    chk(6)
    P.barrier()
    stA.close()

    wgu_sb = [P.sb([128, 8, 2048], BF16, f"wgu_sb{i}") for i in range(2)]
    wd_sb = [P.sb([128, 8, 1024], BF16, f"wd_sb{i}") for i in range(1)]
    bgu_sb = [P.sb([128, 16], F32, f"bgu_sb{i}") for i in range(2)]
    bd_sb = [P.sb([1, 1024], BF16, f"bd_sb{i}") for i in range(2)]
    ones_bf = P.sb([1, 128], BF16, "ones_bf")
    xT_sb = P.sb([128, 8, 1024], BF16, "xT_sb")
    acc_sb = P.sb([128, 8, 1024], F32, "acc_sb")
    Ga_sb = P.sb([128, 8, 32], F32, "Ga_sb")
    actT = [P.sb([128, 8, 512], BF16, f"actT{i}") for i in range(2)]
    gc_r = Ring(P, 2, [128, 512], F32, "gc")
    sg_r = Ring(P, 2, [128, 512], F32, "sg")
    l1_r = Ring(P, 2, [128, 512], F32, "l1")
    P.dve(lambda e: e.memset(ones_bf[:], 1.0), w=["ones_bf"])
    for ex in range(NEXP):
        wg, wgk = wgu_sb[ex % 2], f"wgu{ex % 2}"
        wdd, wdk = wd_sb[0], "wd0"
        for hf in range(2):
            P.dma("pool", lambda e, hf=hf: e.dma_start(out=wg[:, :, hf * 1024:(hf + 1) * 1024],
                                                       in_=wgu[ex * 1024:(ex + 1) * 1024, hf * 1024:(hf + 1) * 1024].rearrange("(k p) n -> p k n", p=128)),
                  w=[wgk])
        P.dma("pool", lambda e: e.dma_start(out=wdd[:], in_=wd[ex * 1024:(ex + 1) * 1024, :].rearrange("(k p) n -> p k n", p=128)), w=[wdk])
        P.dma("sp", lambda e: e.dma_start(out=wgu_bf[ex * 1024:(ex + 1) * 1024, :].rearrange("(k p) n -> p k n", p=128), in_=wg[:]),
              r=[wgk], w=[f"wgubf{ex}"])
        P.dma("act", lambda e: e.dma_start(out=wd_bf[ex * 1024:(ex + 1) * 1024, :].rearrange("(k p) n -> p k n", p=128), in_=wdd[:]),
              r=[wdk], w=[f"wdbf{ex}"])
    ei = 0
    ai = 0
    for sp in range(NT3 // 1024):
        t0 = sp * 1024
        P.dma("sp", lambda e: e.dma_start(out=xT_sb[:], in_=x1T_d[:, t0:t0 + 1024].rearrange("(k p) n -> p k n", p=128)), r=["x1T_d"], w=["xT"])
        P.dma("act", lambda e: e.dma_start(out=Ga_sb[:], in_=G_d[t0:t0 + 1024, :].rearrange("(s p) g -> p s g", p=128)), r=["G_d"], w=["Ga"])
        P.pool(lambda e: e.memset(acc_sb[:], 0.0), w=["acc"])
        for ex in range(NEXP):
            wg, wgk = wgu_sb[ei % 2], f"wgu{ei % 2}"
            wdd, wdk = wd_sb[0], "wd0"
            bg, bgk = bgu_sb[ei % 2], f"bgu{ei % 2}"
            bdd, bdk = bd_sb[ei % 2], f"bd{ei % 2}"
            ei += 1
            for hf in range(2):
                P.dma("sp", lambda e, hf=hf: e.dma_start(out=wg[:, :, hf * 1024:(hf + 1) * 1024],
                                                         in_=wgu_bf[ex * 1024:(ex + 1) * 1024, hf * 1024:(hf + 1) * 1024].rearrange("(k p) n -> p k n", p=128)),
                      r=[f"wgubf{ex}"], w=[wgk])
            P.dma("sp", lambda e: e.dma_start(out=wdd[:], in_=wd_bf[ex * 1024:(ex + 1) * 1024, :].rearrange("(k p) n -> p k n", p=128)),
                  r=[f"wdbf{ex}"], w=[wdk])
            P.dma("sp", lambda e: e.dma_start(out=bg[:], in_=bgu[ex * 128:(ex + 1) * 128, :]), w=[bgk])
            P.dma("pool", lambda e: e.dma_start(out=bdd[:], in_=bd[ex:ex + 1, :]), w=[bdk])
            chk(7)
            for t2 in range(2):
                aT, aTk = actT[ai % 2], f"actT{ai % 2}"
                ai += 1
                for mgi in range(8):
                    pg, pgk = newps()
                    pl, plk = newps()
                    for k in range(8):
                        P.pe(lambda e, k=k: e.matmul(pg[:, :], lhsT=wg[:, k, mgi * 128:(mgi + 1) * 128], rhs=xT_sb[:, k, t2 * 512:(t2 + 1) * 512],
                                                     start=(k == 0), stop=(k == 7)), r=[wgk, "xT"], w=[pgk])
                    for k in range(8):
                        P.pe(lambda e, k=k: e.matmul(pl[:, :], lhsT=wg[:, k, 1024 + mgi * 128:1024 + (mgi + 1) * 128], rhs=xT_sb[:, k, t2 * 512:(t2 + 1) * 512],
                                                     start=(k == 0), stop=(k == 7)), r=[wgk, "xT"], w=[plk])
                    gc, gck = gc_r.next()
                    sg, sgk = sg_r.next()
                    l1, l1k = l1_r.next()
                    P.dve(lambda e: e.tensor_scalar(out=gc[:], in0=pg[:, :], scalar1=bg[:, mgi:mgi + 1], scalar2=LIM, op0=ALU.add, op1=ALU.min),
                          r=[pgk, bgk], w=[gck])
                    P.act(lambda e: e.activation(out=sg[:], in_=gc[:], func=AF.Sigmoid, scale=SWA), r=[gck], w=[sgk])
                    P.dve(lambda e: e.tensor_scalar(out=l1[:], in0=pl[:, :], scalar1=bg[:, 8 + mgi:9 + mgi], scalar2=LIM, op0=ALU.add, op1=ALU.min),
                          r=[plk, bgk], w=[l1k])
                    P.dve(lambda e: e.tensor_scalar(out=l1[:], in0=l1[:], scalar1=-LIM, scalar2=1.0, op0=ALU.max, op1=ALU.add), r=[l1k], w=[l1k])
                    P.dve(lambda e: e.tensor_tensor(out=sg[:], in0=gc[:], in1=sg[:], op=ALU.mult), r=[gck, sgk], w=[sgk])
                    P.dve(lambda e: e.tensor_tensor(out=aT[:, mgi, :], in0=sg[:], in1=l1[:], op=ALU.mult), r=[sgk, l1k], w=[aTk])
                for s in range(4):
                    sub = t2 * 4 + s
                    for half in range(2):
                        py, pyk = newps()
                        for k in range(8):
                            P.pe(lambda e, k=k: e.matmul(py[:, :], lhsT=aT[:, k, s * 128:(s + 1) * 128], rhs=wdd[:, k, half * 512:(half + 1) * 512],
                                                         start=(k == 0), stop=False), r=[aTk, wdk], w=[pyk])
                        P.pe(lambda e: e.matmul(py[:, :], lhsT=ones_bf[0:1, :], rhs=bdd[0:1, half * 512:(half + 1) * 512], start=False, stop=True),
                             r=["ones_bf", bdk], w=[pyk])
                        P.dve(lambda e: e.scalar_tensor_tensor(out=acc_sb[:, sub, half * 512:(half + 1) * 512], in0=py[:, :], scalar=Ga_sb[:, sub, ex:ex + 1],
                                                               in1=acc_sb[:, sub, half * 512:(half + 1) * 512], op0=ALU.mult, op1=ALU.add),
                              r=[pyk, "Ga", "acc"], w=["acc"])
        chk(8)
        for sub in range(8):
            tok0 = t0 + sub * 128
            x1s, x1k = x1s_r.next()
            P.dma("sp", lambda e: e.dma_start(out=x1s[:], in_=x1_d[tok0:tok0 + 128, :]), r=["x1_d"], w=[x1k])
            u, uk = u_r.next()
            P.dve(lambda e: e.scalar_tensor_tensor(out=u[:], in0=x1s[:], scalar=ALPHA, in1=acc_sb[:, sub, :], op0=ALU.mult, op1=ALU.add),
                  r=[x1k, "acc"], w=[uk])
            x2s, x2k = x1s_r.next()
            layernorm(u[:], uk, gb_sb, "gb", 2, x2s[:], x2k, lntmp())
            P.dma("sp", lambda e: e.dma_start(out=o_x2[tok0:tok0 + 128, :], in_=x2s[:]), r=[x2k])
    nc = P.finish()
    return nc, P.stats


def fm16(v):
    return np.ascontiguousarray(v.reshape(16, 128).T)


_PROGS = {}


def _prog(name, fn):
    if name not in _PROGS:
        _PROGS[name] = fn()[0]
    return _PROGS[name]


def kernel(**inputs):
    inp = {k: np.asarray(v) for k, v in inputs.items()}
    x_cur = np.ascontiguousarray(inp["x"], dtype=np.float32)
    idn_bf = np.eye(128, dtype=np.float32).astype(NPBF)
    idn_f = np.eye(128, dtype=np.float32)
    QC = [qcols(0), qcols(1)]
    for l in range(2):
        inp["x_cur"] = x_cur
        r1 = run_bass_kernel_spmd(_prog("p1", build_p1), prep_p1(inp, l, None), core_ids=list(range(8))).results
        r1 = [{k: np.asarray(v) for k, v in r.items()} for r in r1]

        def cat(b, k, axis):
            return np.concatenate([r1[2 * b][k], r1[2 * b + 1][k]], axis=axis)

        m_maps, d_maps = [], []
        for c in range(8):
            b, half = c // 2, c % 2
            qc = QC[half]
            m_maps.append(dict(qn=np.ascontiguousarray(cat(b, "qn", 1)[:, qc]), qr=relayout_qr(cat(b, "qr", 1)[:, qc]),
                               kn=cat(b, "kn", 1), kr=cat(b, "kr", 1), vm=cat(b, "vm", 0), msk=make_masks(half), idn=idn_bf))
            d_maps.append(dict(qd=np.ascontiguousarray(cat(b, "qd", 1)[:, qc]), qi=relayout_qr(cat(b, "qi", 1)[:, qc]),
                               kd=cat(b, "kd", 1), vd=cat(b, "vd", 0), ki=cat(b, "ki", 1),
                               iw=np.ascontiguousarray(cat(b, "iw", 0)[qc]), cmsk=make_cmask(half), idn=idn_bf))
        rm = run_bass_kernel_spmd(_prog("mla", build_mla), m_maps, core_ids=list(range(8))).results
        rd = run_bass_kernel_spmd(_prog("dsa", build_dsa), d_maps, core_ids=list(range(8))).results
        od_full, oc_full = [], []
        for b in range(4):
            om = np.zeros((512, S), NPBF)
            oc_ = np.zeros((512, S), NPBF)
            for half in range(2):
                om[:, QC[half]] = np.asarray(rm[2 * b + half]["o"])
                oc_[:, QC[half]] = np.asarray(rd[2 * b + half]["o"])
            od_full.append(om)
            oc_full.append(oc_)
        lnv = np.concatenate([inp["ln1_g"][l], inp["ln1_b"][l], inp["ln2_g"][l], inp["ln2_b"][l]]).reshape(1, 4096).astype(np.float32)
        bgu = np.ascontiguousarray(np.stack([fm16(inp["exp_b_gu"][l][e]) for e in range(32)]).reshape(32 * 128, 16))
        wgu = np.ascontiguousarray(inp["exp_w_gu"][l].reshape(32 * 1024, 2048))
        wd = np.ascontiguousarray(inp["exp_w_d"][l].reshape(32 * 1024, 1024))
        maps3 = []
        for c in range(2):
            bs = (2 * c, 2 * c + 1)
            maps3.append(dict(
                ma=np.concatenate([cat(b, "ma", 1) for b in bs], axis=1), mb=np.concatenate([cat(b, "mb", 1) for b in bs], axis=1),
                g2=np.concatenate([cat(b, "g2", 1) for b in bs], axis=1), g3=np.concatenate([cat(b, "g3", 1) for b in bs], axis=1),
                oc=np.concatenate([oc_full[b] for b in bs], axis=1), od=np.concatenate([od_full[b] for b in bs], axis=1),
                x=np.ascontiguousarray(x_cur[2 * c:2 * c + 2].reshape(NT3, 1024)),
                dsaout=inp["dsa_out"][l], mlaout=inp["mla_out"][l], wo=inp["w_o"][l], lnv=lnv,
                rw=inp["router_w"][l], rb=inp["router_b"][l].reshape(1, 32), wgu=wgu, bgu=bgu, wd=wd, bd=inp["exp_b_d"][l], idn=idn_f))
        del r1, rm, rd
        r3 = run_bass_kernel_spmd(_prog("p3", build_p3), maps3, core_ids=[0, 1]).results
        x_cur = np.concatenate([np.asarray(r3[0]["x2"]), np.asarray(r3[1]["x2"])], axis=0).reshape(4, S, 1024).astype(np.float32)
        del maps3, r3
    return x_cur
```
